# Optimizing a Trainium2 kernel written in Bass

```python
import jax, jax.numpy as jnp
from jax import lax
import numpy as np

D_MODEL = 2048
BATCH = 32
SEQ = 256
DEPTH = 4
DEC_BATCH = 8
DEC_SEQ = 1024
PAST_LEN = 512

GRID_W = 64
N_MIXERS = 3
N_MLSTM_LAYERS = (DEPTH + 2) // 3
N_FOURIER_LAYERS = (DEPTH + 1) // 3
N_LRU_LAYERS = DEPTH // 3
MLSTM_HEADS = 4
MLSTM_DK = D_MODEL // 8
MLSTM_DV = D_MODEL // 4
MLSTM_CHUNK = 64
FNET_GROUPS = 8
FNET_GC = D_MODEL // FNET_GROUPS
LRU_WIDTH = D_MODEL
LRU_BLOCKS = 8
LRU_BS = LRU_WIDTH // LRU_BLOCKS
CONV_W = 4
LRU_C = 8.0
D_FF = 4 * D_MODEL
EPS = 1e-6

kernel_name = 'hybrid_mlstm_fnet_rglru_diffusion_step'


def rms_norm(x, g):
    xf = x.astype(jnp.float32)
    y = xf * lax.rsqrt(jnp.mean(xf * xf, axis=-1, keepdims=True) + EPS)
    return (y * g.astype(jnp.float32)).astype(x.dtype)


def modulation(cond, w, b):
    m = jax.nn.silu(cond) @ w + b
    return jnp.split(m[..., None, :], 6, axis=-1)


def _flip(a):
    return jnp.flip(a, axis=1)


def mlstm_chunkwise(q, k, v, log_i, log_f, C0, n0, m0):
    B, T, H, _ = q.shape
    L = MLSTM_CHUNK
    nc = T // L

    def to_chunks(a):
        return jnp.moveaxis(a.reshape((B, nc, L) + a.shape[2:]), 1, 0)

    causal = jnp.tril(jnp.ones((L, L), dtype=bool))

    def step(carry, inp):
        C, n, m = carry
        qc, kc, vc, li, lf = inp
        b = jnp.cumsum(lf, axis=1)
        Dm = b[:, :, None, :] - b[:, None, :, :] + li[:, None, :, :]
        Dm = jnp.where(causal[None, :, :, None], Dm, -jnp.inf)
        inter = b + m[:, None, :]
        m_t = jnp.maximum(inter, jnp.max(Dm, axis=2))
        w_inter = jnp.exp(inter - m_t)
        s = jnp.einsum('bthd,bshd->btsh', qc, kc) * jnp.exp(Dm - m_t[:, :, None, :])
        num = jnp.einsum('btsh,bshv->bthv', s, vc) + w_inter[..., None] * jnp.einsum('bthd,bhdv->bthv', qc, C)
        den = jnp.sum(s, axis=2) + w_inter * jnp.einsum('bthd,bhd->bth', qc, n)
        h = num / jnp.maximum(jnp.abs(den), jnp.exp(-m_t))[..., None]
        bL = b[:, -1, :]
        g = bL[:, None, :] - b + li
        m_new = jnp.maximum(bL + m, jnp.max(g, axis=1))
        wC = jnp.exp(bL + m - m_new)
        ws = jnp.exp(g - m_new[:, None, :])
        C_new = wC[..., None, None] * C + jnp.einsum('bsh,bshd,bshv->bhdv', ws, kc, vc)
        n_new = wC[..., None] * n + jnp.einsum('bsh,bshd->bhd', ws, kc)
        return (C_new, n_new, m_new), h

    (C, n, m), hs = lax.scan(step, (C0, n0, m0), tuple(map(to_chunks, (q, k, v, log_i, log_f))))
    h = jnp.moveaxis(hs, 0, 1).reshape(B, T, H, v.shape[-1])
    return h, C, n, m


def mlstm_mixer(h, w_in, b_gate, norm_g, w_out, C0, n0, m0):
    B, T, _ = h.shape
    f32 = jnp.float32
    HK = MLSTM_HEADS * MLSTM_DK
    HV = MLSTM_HEADS * MLSTM_DV
    q, k, v, o, g = jnp.split(h @ w_in, [HK, 2 * HK, 2 * HK + HV, 2 * HK + 2 * HV], axis=-1)
    q = q.astype(f32).reshape(B, T, MLSTM_HEADS, MLSTM_DK)
    k = k.astype(f32).reshape(B, T, MLSTM_HEADS, MLSTM_DK) * (MLSTM_DK ** -0.5)
    v = v.astype(f32).reshape(B, T, MLSTM_HEADS, MLSTM_DV)
    g = g.astype(f32).reshape(B, T, 2, 2, MLSTM_HEADS) + b_gate.astype(f32)
    hs, Cs, ns, ms = [], [], [], []
    for d in range(2):
        li = g[:, :, d, 0]
        lf = jax.nn.log_sigmoid(g[:, :, d, 1])
        args = (q, k, v, li, lf)
        if d == 1:
            args = tuple(map(_flip, args))
        hd, C, n, m = mlstm_chunkwise(*args, C0[:, d].astype(f32), n0[:, d].astype(f32), m0[:, d].astype(f32))
        hs.append(_flip(hd) if d == 1 else hd)
        Cs.append(C); ns.append(n); ms.append(m)
    hsum = hs[0] + hs[1]
    hsum = hsum * lax.rsqrt(jnp.mean(hsum * hsum, axis=-1, keepdims=True) + EPS)
    y = hsum.reshape(B, T, HV) * norm_g.astype(f32) * jax.nn.sigmoid(o.astype(f32))
    out = y.astype(h.dtype) @ w_out
    return out, jnp.stack(Cs, 1), jnp.stack(ns, 1), jnp.stack(ms, 1)


def fourier_mixer(h, w_out, b_out):
    B, T, D = h.shape
    hg = h.astype(jnp.float32).reshape(B, T, FNET_GROUPS, FNET_GC)
    mixed = jnp.fft.fft2(hg, axes=(1, 3), norm='ortho').real
    return mixed.reshape(B, T, D).astype(h.dtype) @ w_out + b_out


def depthwise_conv_centred(x, w, b):
    pad_l = (CONV_W - 1) // 2
    y = lax.conv_general_dilated(x, w[:, None, :].astype(x.dtype), (1,), [(pad_l, CONV_W - 1 - pad_l)],
                                 dimension_numbers=('NWC', 'WIO', 'NWC'), feature_group_count=x.shape[-1])
    return y + b


def _lin_combine(e1, e2):
    a1, b1 = e1
    a2, b2 = e2
    return a1 * a2, a2 * b1 + b2


def rglru_mixer(h, w_in, conv_w, conv_b, gate_w, gate_b, lam, w_out, h0):
    B, T, _ = h.shape
    f32 = jnp.float32
    gate_br, xb = jnp.split(h @ w_in, 2, axis=-1)
    xb = depthwise_conv_centred(xb, conv_w, conv_b).astype(f32)
    pre = jnp.einsum('btnk,dgnkj->btdgnj', xb.reshape(B, T, LRU_BLOCKS, LRU_BS), gate_w.astype(f32))
    pre = pre.reshape(B, T, 2, 2, LRU_WIDTH) + gate_b.astype(f32)
    r = jax.nn.sigmoid(pre[:, :, :, 0])
    ig = jax.nn.sigmoid(pre[:, :, :, 1])
    log_a = LRU_C * r * jax.nn.log_sigmoid(lam.astype(f32))
    a = jnp.exp(log_a)
    u = jnp.sqrt(-jnp.expm1(2.0 * log_a)) * (ig * xb[:, :, None, :])
    a = jnp.stack([a[:, :, 0], _flip(a[:, :, 1])], axis=2)
    u = jnp.stack([u[:, :, 0], _flip(u[:, :, 1])], axis=2)
    u = u.at[:, 0].add(a[:, 0] * h0.astype(f32))
    _, hs = lax.associative_scan(_lin_combine, (a, u), axis=1)
    y = hs[:, :, 0] + _flip(hs[:, :, 1])
    out = (y * jax.nn.gelu(gate_br.astype(f32))).astype(h.dtype) @ w_out
    return out, hs[:, -1]


def sq_relu_mlp(h, w_up, w_down):
    return jnp.square(jax.nn.relu(h @ w_up)) @ w_down


def trunk(x, cond, C0, n0, m0, h0, mod_w, mod_b, norm_g, ffn_w_up, ffn_w_down,
          mlstm_w_in, mlstm_b_gate, mlstm_norm_g, mlstm_w_out, fnet_w_out, fnet_b_out,
          lru_w_in, lru_conv_w, lru_conv_b, lru_gate_w, lru_gate_b, lru_lambda, lru_w_out):
    Cs, ns, ms, hs = [], [], [], []
    for i in range(DEPTH):
        sh1, sc1, g1, sh2, sc2, g2 = modulation(cond, mod_w[i], mod_b[i])
        hin = rms_norm(x, norm_g[i, 0]) * (1 + sc1) + sh1
        kind, j = i % N_MIXERS, i // N_MIXERS
        if kind == 0:
            out, C, n, m = mlstm_mixer(hin, mlstm_w_in[j], mlstm_b_gate[j], mlstm_norm_g[j], mlstm_w_out[j],
                                       C0[:, j], n0[:, j], m0[:, j])
            Cs.append(C); ns.append(n); ms.append(m)
        elif kind == 1:
            out = fourier_mixer(hin, fnet_w_out[j], fnet_b_out[j])
        else:
            out, hl = rglru_mixer(hin, lru_w_in[j], lru_conv_w[j], lru_conv_b[j], lru_gate_w[j], lru_gate_b[j],
                                  lru_lambda[j], lru_w_out[j], h0[:, j])
            hs.append(hl)
        x = x + g1 * rms_norm(out.astype(x.dtype), norm_g[i, 1])
        hin = rms_norm(x, norm_g[i, 2]) * (1 + sc2) + sh2
        x = x + g2 * rms_norm(sq_relu_mlp(hin, ffn_w_up[i], ffn_w_down[i]).astype(x.dtype), norm_g[i, 3])
    return x, jnp.stack(Cs, 1), jnp.stack(ns, 1), jnp.stack(ms, 1), jnp.stack(hs, 1)


def setup_inputs(seed: int = 0) -> dict:
    key = jax.random.key(seed)
    ks = jax.random.split(key, 32)
    f32 = jnp.float32

    def nrm(k, shape, s):
        return jax.random.normal(k, shape, f32) * s

    D = D_MODEL
    H = MLSTM_HEADS
    HK = H * MLSTM_DK
    HV = H * MLSTM_DV
    NA, NB, NC = N_MLSTM_LAYERS, N_FOURIER_LAYERS, N_LRU_LAYERS
    gate_base = jnp.stack([jnp.zeros((H,), f32), jnp.linspace(3.0, 6.0, H, dtype=f32)])
    a_target = jax.random.uniform(ks[24], (NC, 2, LRU_WIDTH), f32, 0.9, 0.999)
    a0 = a_target ** (1.0 / LRU_C)
    return {
        'x_prompt': nrm(ks[0], (BATCH, SEQ, D), 1.0),
        'x_sample': nrm(ks[1], (DEC_BATCH, DEC_SEQ, D), 1.0),
        'c': nrm(ks[2], (DEC_BATCH, D), 1.0),
        'state_mlstm_C': nrm(ks[3], (DEC_BATCH, NA, 2, H, MLSTM_DK, MLSTM_DV), 1.0),
        'state_mlstm_n': nrm(ks[4], (DEC_BATCH, NA, 2, H, MLSTM_DK), 1.0),
        'state_mlstm_m': nrm(ks[5], (DEC_BATCH, NA, 2, H), 1.0),
        'state_lru_h': nrm(ks[6], (DEC_BATCH, NC, 2, LRU_WIDTH), 1.0),
        'c_ctx': nrm(ks[7], (D,), 1.0),
        'mod_w': nrm(ks[8], (DEPTH, D, 6 * D), 0.5 * D ** -0.5),
        'mod_b': nrm(ks[9], (DEPTH, 6 * D), 0.05),
        'norm_g': 1.0 + nrm(ks[10], (DEPTH, 4, D), 0.1),
        'ffn_w_up': nrm(ks[11], (DEPTH, D, D_FF), D ** -0.5),
        'ffn_w_down': nrm(ks[12], (DEPTH, D_FF, D), D_FF ** -0.5),
        'mlstm_w_in': nrm(ks[13], (NA, D, 2 * HK + 2 * HV + 4 * H), D ** -0.5),
        'mlstm_b_gate': nrm(ks[14], (NA, 2, 2, H), 0.1) + gate_base[None, None],
        'mlstm_norm_g': 1.0 + nrm(ks[15], (NA, HV), 0.1),
        'mlstm_w_out': nrm(ks[16], (NA, HV, D), HV ** -0.5),
        'fnet_w_out': nrm(ks[17], (NB, D, D), D ** -0.5),
        'fnet_b_out': nrm(ks[18], (NB, D), 0.02),
        'lru_w_in': nrm(ks[19], (NC, D, 2 * LRU_WIDTH), D ** -0.5),
        'lru_conv_w': nrm(ks[20], (NC, CONV_W, LRU_WIDTH), CONV_W ** -0.5),
        'lru_conv_b': nrm(ks[21], (NC, LRU_WIDTH), 0.02),
        'lru_gate_w': nrm(ks[22], (NC, 2, 2, LRU_BLOCKS, LRU_BS, LRU_BS), LRU_BS ** -0.5),
        'lru_gate_b': nrm(ks[23], (NC, 2, 2, LRU_WIDTH), 0.02),
        'lru_lambda': jnp.log(a0) - jnp.log1p(-a0),
        'lru_w_out': nrm(ks[25], (NC, LRU_WIDTH, D), LRU_WIDTH ** -0.5),
    }


def reference(x_prompt, x_sample, c, state_mlstm_C, state_mlstm_n, state_mlstm_m, state_lru_h, c_ctx,
              mod_w, mod_b, norm_g, ffn_w_up, ffn_w_down, mlstm_w_in, mlstm_b_gate, mlstm_norm_g, mlstm_w_out,
              fnet_w_out, fnet_b_out, lru_w_in, lru_conv_w, lru_conv_b, lru_gate_w, lru_gate_b, lru_lambda,
              lru_w_out):
    weights = (mod_w, mod_b, norm_g, ffn_w_up, ffn_w_down, mlstm_w_in, mlstm_b_gate, mlstm_norm_g, mlstm_w_out,
               fnet_w_out, fnet_b_out, lru_w_in, lru_conv_w, lru_conv_b, lru_gate_w, lru_gate_b, lru_lambda,
               lru_w_out)
    Bp = x_prompt.shape[0]
    f32 = jnp.float32
    zC = jnp.zeros((Bp, N_MLSTM_LAYERS, 2, MLSTM_HEADS, MLSTM_DK, MLSTM_DV), f32)
    zn = jnp.zeros((Bp, N_MLSTM_LAYERS, 2, MLSTM_HEADS, MLSTM_DK), f32)
    zm = jnp.zeros((Bp, N_MLSTM_LAYERS, 2, MLSTM_HEADS), f32)
    zh = jnp.zeros((Bp, N_LRU_LAYERS, 2, LRU_WIDTH), f32)
    y_prompt, new_C, new_n, new_m, new_h = trunk(x_prompt, c_ctx, zC, zn, zm, zh, *weights)
    y_sample, _, _, _, _ = trunk(x_sample, c, state_mlstm_C, state_mlstm_n, state_mlstm_m, state_lru_h, *weights)
    return (y_prompt, y_sample, new_C, new_n, new_m, new_h)
```

```python
import math
import os
import numpy as np
import concourse.bass as bass
import concourse.mybir as mybir
from concourse.bass_utils import run_bass_kernel_spmd

F32 = mybir.dt.float32
BF16 = mybir.dt.bfloat16
U8 = mybir.dt.uint8
AF = mybir.ActivationFunctionType
ALU = mybir.AluOpType

D = 2048
KC = 16
TOK = 1024
DEPTH = 4
EPS = 1e-6
KiB = 1024
OFF_X = 0
OFF_H = 64 * KiB
OFF_S = 96 * KiB
OFF_W = 152 * KiB
OFF_C = 200 * KiB
ARENA = 212000
WBUF_BYTES = 16 * KiB
NWBUF = 3


def _esz(dt):
    return {F32: 4, BF16: 2, U8: 1}[dt]


_BIG = {
    "mod_w": [4, D, 6 * D], "ffn_w_up": [4, D, 4 * D], "ffn_w_down": [4, 4 * D, D],
    "mlstm_w_in": [2, D, 6160], "mlstm_w_out": [2, D, D], "fnet_w_out": [1, D, D],
    "lru_w_in": [1, D, 2 * D], "lru_gate_w": [1, 2, 2, 8, 256, 256], "lru_w_out": [1, D, D],
    "c_dft1024": [1024, 2048], "stC": [2, 2, 4, 256, 512],
}


def _big_shapes(cfg):
    cfg = cfg or {}
    sh = {kx: list(v) for kx, v in _BIG.items()}
    if not cfg.get("slim"):
        return sh
    nl = cfg.get("nlayers", DEPTH)
    sam = cfg.get("stop_after_mixer", False)
    passes = cfg.get("passes", (0, 1))
    skipmix = cfg.get("skip_mixer", False)
    sh["mod_w"][0] = 1 if cfg.get("skip_mod") else nl
    nffn = nl - 1 if sam else nl
    for n_ in ("ffn_w_up", "ffn_w_down"):
        sh[n_][0] = max(nffn, 1)
        if nffn == 0:
            sh[n_][1] = 128
    nml = 0 if skipmix else (nl + 2) // 3
    for n_ in ("mlstm_w_in", "mlstm_w_out"):
        sh[n_][0] = max(nml, 1)
        if nml == 0:
            sh[n_][1] = 128
    if skipmix or nl < 2:
        sh["fnet_w_out"][1] = 128
    if skipmix or nl < 3:
        sh["lru_w_in"][1] = 128
        sh["lru_w_out"][1] = 128
        sh["lru_gate_w"][3] = 1
    if skipmix or nl < 2 or 0 not in passes:
        sh["c_dft1024"][0] = 128
    if skipmix or 0 not in passes:
        sh["stC"][0] = 1
        sh["stC"][3] = 128
    if cfg.get("skip_mod"):
        sh["mod_w"][1] = 128
    return sh


def _slice_to(a, shape):
    return np.ascontiguousarray(a[tuple(slice(0, s) for s in shape)])


class _Eng:
    def __init__(self, name, h, skip_self=False):
        self.name = name
        self.h = h
        self.sem = None
        self.key = None
        self.cnt = 0
        self.seen = {}
        self.ownkeys = set()
        self.skip_self = skip_self


class _Slot:
    def __init__(self, sem, key):
        self.sem = sem
        self.key = key
        self.val = 0


class KB:
    PAGE = 64
    SEMCAP = 30000

    def __init__(self, marked=None):
        self.nc = bass.Bass("TRN2", target_bir_lowering=False)
        nc = self.nc
        self.marked = marked
        self.used = {}
        self.engkeys = set()
        self.rankmap = None
        if marked is not None:
            self.rankmap = {kx: {idx: i + 1 for i, idx in enumerate(sorted(v))} for kx, v in marked.items()}
        self.arena = nc.alloc_sbuf_tensor("arena", [128, ARENA], U8)
        self.arena_base = nc.lookup_mloc(self.arena).addr
        self.base = {}
        self.pages = {}
        self.nsem = 0
        self.eng = {
            "pe": _Eng("pe", nc.tensor, skip_self=True),
            "act": _Eng("act", nc.scalar),
            "dve": _Eng("dve", nc.vector),
            "pool": _Eng("pool", nc.gpsimd),
            "sp": _Eng("sp", nc.sync),
        }
        for e in ("pe", "act", "dve", "pool"):
            self._new_epoch(self.eng[e])
        self.slots = {
            "sp": [self._mkslot() for _ in range(16)],
            "pool": [self._mkslot() for _ in range(8)],
        }
        self.dma_i = {"sp": 0, "pool": 0}
        self.views = {}
        self.banks = []
        for i in range(8):
            p = nc.alloc_psum_tensor(f"pb{i}", [128, 512], F32)
            ml = nc.lookup_mloc(p)
            self.base[p.name] = ("PS", ml.bank * 2048 + ml.addr)
            self.banks.append(p)
        assert len({self.base[p.name][1] for p in self.banks}) == 8
        self.bank_i = 0
        self.nbanks = 8
        self.wi = 0
        self.stages = []

    def _newsem(self):
        self.nsem += 1
        name = f"s{self.nsem}"
        return self.nc.alloc_semaphore(name), name

    def _mkslot(self):
        s, kx = self._newsem()
        return _Slot(s, kx)

    def _new_epoch(self, e):
        e.sem, e.key = self._newsem()
        e.ownkeys.add(e.key)
        self.engkeys.add(e.key)
        e.cnt = 0

    def _semval(self, kx, val):
        if kx in self.engkeys:
            self.used.setdefault(kx, set()).add(val)
            if self.rankmap is not None:
                return self.rankmap[kx][val]
        return val

    def view(self, off, shape, dt):
        kx = (off, tuple(shape), dt)
        if kx not in self.views:
            h = self.nc.alloc_sbuf_tensor_at(f"v{len(self.views)}", list(shape), dt, offset=self.arena_base + off)
            self.base[h.name] = ("SB", self.arena_base + off)
            self.views[kx] = h
        return self.views[kx]

    def bank(self):
        b = self.banks[self.bank_i % self.nbanks]
        self.bank_i += 1
        return b

    def bank_bf(self):
        return self.as_bf(self.bank())

    def as_bf(self, b):
        hb = b.bitcast(BF16)
        self.base[hb.name] = self.base[b.name]
        return hb

    def wbuf(self, shape, dt=BF16):
        i = self.wi % NWBUF
        self.wi += 1
        return self.view(OFF_W + i * WBUF_BYTES, shape, dt)

    def _rng(self, ap):
        t = ap.tensor
        if t.name not in self.base:
            return None
        space, base = self.base[t.name]
        esz = _esz(ap.dtype)
        shape = list(t.shape)
        pstride = 1
        for s in shape[1:]:
            pstride *= s
        off = ap.offset % pstride
        lo = hi = off
        dims = list(ap.ap)
        for (step, cnt) in dims[1:]:
            ext = step * (cnt - 1)
            if ext < 0:
                lo += ext
            else:
                hi += ext
        return (space, base + lo * esz, base + (hi + 1) * esz)

    def _pagekeys(self, r):
        if r[0] == "K":
            return [r]
        space, lo, hi = r
        P = 2048 if space == "PS" else self.PAGE
        return [(space, pg) for pg in range(lo // P, (hi - 1) // P + 1)]

    def _deps(self, reads, writes):
        deps = {}

        def add(rec):
            if rec is None:
                return
            o = deps.get(rec[1])
            if o is None or o[2] < rec[2]:
                deps[rec[1]] = rec

        for r in reads:
            for pk in self._pagekeys(r):
                st = self.pages.get(pk)
                if st is not None:
                    add(st[0])
        for r in writes:
            for pk in self._pagekeys(r):
                st = self.pages.get(pk)
                if st is not None:
                    add(st[0])
                    for rec in st[1].values():
                        add(rec)
        return deps

    def _commit(self, reads, writes, rec):
        for r in reads:
            for pk in self._pagekeys(r):
                st = self.pages.get(pk)
                if st is None:
                    st = [None, {}]
                    self.pages[pk] = st
                st[1][rec[1]] = rec
        for r in writes:
            for pk in self._pagekeys(r):
                self.pages[pk] = [rec, {}]

    def _wait(self, e, deps):
        for kx, (sem, _, val) in deps.items():
            if kx in e.ownkeys:
                if e.skip_self or kx != e.key or val < e.cnt - 1:
                    continue
            if e.seen.get(kx, 0) >= val:
                continue
            e.h.wait_ge(sem, self._semval(kx, val))
            e.seen[kx] = val

    def op(self, eng, name, **kw):
        e = self.eng[eng]
        if e.cnt >= self.SEMCAP:
            self._new_epoch(e)
        reads, writes = [], []
        for kx, v in kw.items():
            if hasattr(v, "tensor") and hasattr(v, "ap"):
                r = self._rng(v)
                if r is None:
                    continue
                if kx in ("out", "accum_out", "ap"):
                    writes.append(r)
                else:
                    reads.append(r)
        self._wait(e, self._deps(reads, writes))
        ins = getattr(e.h, name)(**kw)
        e.cnt += 1
        if self.marked is None or e.cnt in self.marked.get(e.key, ()):
            ins.then_inc(e.sem, 1)
        self._commit(reads, writes, (e.sem, e.key, e.cnt))
        return ins

    def dma(self, issuer, out, in_, rkeys=(), wkeys=(), **kw):
        e = self.eng[issuer]
        reads = [("K", x) for x in rkeys]
        writes = [("K", x) for x in wkeys]
        r = self._rng(in_)
        if r is not None:
            reads.append(r)
        r = self._rng(out)
        if r is not None:
            writes.append(r)
        self._wait(e, self._deps(reads, writes))
        pool = self.slots[issuer]
        slot = pool[self.dma_i[issuer] % len(pool)]
        self.dma_i[issuer] += 1
        if slot.val > 0 and e.seen.get(slot.key, 0) < slot.val:
            e.h.wait_ge(slot.sem, slot.val)
            e.seen[slot.key] = slot.val
        ins = e.h.dma_start(out=out, in_=in_, **kw)
        slot.val += 16
        ins.then_inc(slot.sem, 16)
        self._commit(reads, writes, (slot.sem, slot.key, slot.val))

    def finish(self):
        e = self.eng["sp"]
        for pool in self.slots.values():
            for s in pool:
                if s.val > 0:
                    e.h.wait_ge(s.sem, s.val)
        for n in ("pe", "act", "dve", "pool"):
            x = self.eng[n]
            if x.cnt > 0:
                e.h.wait_ge(x.sem, self._semval(x.key, x.cnt))

    def stage(self, load, compute):
        self.stages.append((load, compute))

    def run(self, depth=2):
        st = self.stages
        n = len(st)
        load_idx = [i for i in range(n) if st[i][0] is not None]
        hs = {}
        nl = 0
        cdone = 0
        for i in range(n):
            while nl < len(load_idx) and nl < cdone + NWBUF:
                li = load_idx[nl]
                hs[li] = st[li][0]()
                nl += 1
            st[i][1](hs.pop(i, None))
            if st[i][0] is not None:
                cdone += 1
        self.stages = []


def build_program(cfg=None):
    k1 = _emit_program(cfg, None)
    k2 = _emit_program(cfg, k1.used)
    return k2.nc


def _emit_program(cfg=None, marked=None):
    cfg = cfg or {}
    nlayers = cfg.get("nlayers", DEPTH)
    passes = cfg.get("passes", (0, 1))
    stop_after_mixer = cfg.get("stop_after_mixer", False)
    k = KB(marked)
    nc = k.nc

    def din(name, shape):
        return nc.dram_tensor(name, list(shape), F32, kind="ExternalInput").ap()

    bsh = _big_shapes(cfg)

    def dout(name, shape):
        return nc.dram_tensor(name, list(shape), F32, kind="ExternalOutput").ap()

    xT_in = [din("xT_a", [D, TOK]), din("xT_b", [D, TOK])]
    cond_d = din("cond", [2, 128, 16])
    stC_d = din("stC", bsh["stC"])
    stn_d = din("stn", [2, 2, 4, 128, 2])
    stm_d = din("stm", [2, 2, 4])
    stmT_d = din("stmT", [2, 4, 2])
    sth_d = din("sth", [2, 128, 16])
    mod_w = din("mod_w", bsh["mod_w"])
    mod_b = din("mod_b_fm", [128, 4 * 96])
    norm_g = din("norm_g_fm", [128, 4 * 4 * 16])
    ffn_up = din("ffn_w_up", bsh["ffn_w_up"])
    ffn_dn = din("ffn_w_down", bsh["ffn_w_down"])
    ml_win = din("mlstm_w_in", bsh["mlstm_w_in"])
    ml_bg = din("mlstm_bg", [2, 4, 4])
    ml_ng = din("mlstm_ng_fm", [2, 128, 16])
    ml_ngrow = din("mlstm_ng_row", [2, D])
    ml_wout = din("mlstm_w_out", bsh["mlstm_w_out"])
    fn_wout = din("fnet_w_out", bsh["fnet_w_out"])
    fn_b = din("fnet_b_fm", [128, 16])
    lru_win = din("lru_w_in", bsh["lru_w_in"])
    lru_cw = din("lru_cw_fm", [128, 4 * 16])
    lru_cb = din("lru_cb_fm", [128, 16])
    lru_gw = din("lru_gate_w", bsh["lru_gate_w"])
    lru_gb = din("lru_gb_fm", [128, 4 * 16])
    lru_lam = din("lru_lam_fm", [128, 2 * 16])
    lru_wout = din("lru_w_out", bsh["lru_w_out"])
    c_ident = din("c_ident", [128, 128])
    c_cs256 = din("c_cs256", [256, 512])
    c_dft1024 = din("c_dft1024", bsh["c_dft1024"])
    c_dft256 = din("c_dft256", [256, 512])
    c_mask = din("c_mask", [2, 128, 128])
    c_sel4 = din("c_sel4", [4, 4 * 128])
    c_id4 = din("c_id4", [4, 4])

    yT_out = [dout("yT_a", [D, TOK]), dout("yT_b", [D, TOK])]
    newC_o = dout("newC", [4, 2, 2, 4, 256, 512])
    newn_o = dout("newn", [4, 2, 2, 4, 128, 2])
    newm_o = dout("newm", [2, 4, 8])
    newh_o = dout("newh", [4, 2, 128, 16])
    xs_d = nc.dram_tensor("xs_scratch", [D, TOK], F32).ap()

    xT = k.view(OFF_X, [128, KC, TOK], F32)
    hinT = k.view(OFF_H, [128, KC, TOK], BF16)
    outT_alt = k.view(OFF_H, [128, KC, TOK], F32)
    co = [OFF_C]

    def calloc(shape, dt):
        n = 1
        for s in shape[1:]:
            n *= s
        nb = (n * _esz(dt) + 31) // 32 * 32
        v = k.view(co[0], shape, dt)
        co[0] += nb
        assert co[0] <= ARENA
        return v

    modv = calloc([128, 4, 96, 2], F32)
    modb = calloc([128, 4, 96], F32)
    ng = calloc([128, 4, 4, 16], F32)
    der = calloc([128, 4, 16], F32)
    ident = calloc([128, 128], BF16)
    onesD = calloc([128, 128], BF16)
    ones1 = calloc([128, 2], BF16)
    condT = calloc([128, 2, 16], F32)
    scT = calloc([128, 16, 2], BF16)
    fbias = calloc([128, 16], F32)
    NT = OFF_S + 32 * KiB
    sqb = [k.view(NT + i * KiB, [128, 512], BF16) for i in range(2)]
    rsb = [k.view(NT + 2 * KiB + i * 2 * KiB, [128, 512], F32) for i in range(2)]
    rsb2 = [k.view(NT + 6 * KiB + i * 2 * KiB, [128, 512], F32) for i in range(2)]
    tmpb = [k.view(NT + 10 * KiB + i * 2 * KiB, [128, 512], F32) for i in range(3)]
    xstage = [k.view(NT + 16 * KiB + i * 2 * KiB, [128, 512], F32) for i in range(4)]

    k.dma("pool", out=ident[:], in_=c_ident)
    k.dma("sp", out=modb[:], in_=mod_b.rearrange("p (l c) -> p l c", l=4))
    k.dma("sp", out=ng[:], in_=norm_g.rearrange("p (l a c) -> p l a c", l=4, a=4))
    k.dma("sp", out=condT[:], in_=cond_d.rearrange("r p c -> p r c"))
    k.op("dve", "memset", ap=onesD[:], constant=1.0 / D)
    k.op("dve", "memset", ap=ones1[:], constant=1.0)
    for r in range(2):
        k.op("act", "activation", out=scT[:, :, r], in_=condT[:, r, :], func=AF.Silu)


    def mod_stages(l):
        out = []
        for g in range(24):
            def load(l=l, g=g):
                wb = k.wbuf([128, KC, 512])
                k.dma("pool", out=wb[:], in_=mod_w[l, :, g * 512:(g + 1) * 512].rearrange("(k p) m -> p k m", p=128))
                return wb

            def comp(wb, l=l, g=g):
                for mc in range(4):
                    pb = k.bank()
                    for kc in range(KC):
                        k.op("pe", "matmul", out=pb[:, 0:2], lhsT=wb[:, kc, mc * 128:(mc + 1) * 128], rhs=scT[:, kc, :],
                             start=(kc == 0), stop=(kc == KC - 1))
                    c = g * 4 + mc
                    k.op("act", "activation", out=modv[:, l, c, :], in_=pb[:, 0:2], func=AF.Identity,
                         bias=modb[:, l, c:c + 1])
            out.append((load, comp))
        return out

    if not cfg.get('skip_mod'):
        for st_ in mod_stages(0):
            k.stage(*st_)

    def stats_rstd(src, b):
        blk = slice(b * 512, (b + 1) * 512)
        pb = k.bank()
        for c in range(KC):
            sq = sqb[c % 2]
            k.op("act", "activation", out=sq[:], in_=src[:, c, blk], func=AF.Square)
            k.op("pe", "matmul", out=pb[:], lhsT=onesD[:], rhs=sq[:], start=(c == 0), stop=(c == KC - 1))
        k.op("act", "activation", out=rsb[b][:], in_=pb[:], func=AF.Ln, bias=EPS, scale=1.0)
        k.op("act", "activation", out=rsb[b][:], in_=rsb[b][:], func=AF.Exp, scale=-0.5)

    SB = [k.banks[6], k.banks[7]]
    sst = [0]

    def src_stat(c, b, ap):
        if c == 0 and b == 0:
            k.nbanks = 6
        sq = sqb[sst[0] % 2]
        sst[0] += 1
        k.op("act", "activation", out=sq[:], in_=ap, func=AF.Square)
        k.op("pe", "matmul", out=SB[b][:], lhsT=onesD[:], rhs=sq[:], start=(c == 0), stop=(c == KC - 1))

    def derive(l, r):
        mv = modv
        k.op("dve", "scalar_tensor_tensor", out=der[:, 0, :], in0=mv[:, l, 16:32, r], scalar=1.0, in1=ng[:, l, 0, :],
             op0=ALU.add, op1=ALU.mult)
        k.op("dve", "tensor_tensor", out=der[:, 1, :], in0=mv[:, l, 32:48, r], in1=ng[:, l, 1, :], op=ALU.mult)
        k.op("dve", "scalar_tensor_tensor", out=der[:, 2, :], in0=mv[:, l, 64:80, r], scalar=1.0, in1=ng[:, l, 2, :],
             op0=ALU.add, op1=ALU.mult)
        k.op("dve", "tensor_tensor", out=der[:, 3, :], in0=mv[:, l, 80:96, r], in1=ng[:, l, 3, :], op=ALU.mult)

    def prenorm(l, r, which, have_stats=False):
        ai = 0 if which == 0 else 2
        b0 = 0 if which == 0 else 48
        for b in range(2):
            blk = slice(b * 512, (b + 1) * 512)
            if have_stats:
                rs_ = rsb2[b]
            else:
                stats_rstd(xT, b)
                rs_ = rsb[b]
            for c in range(KC):
                tb = tmpb[c % 3]
                k.op("dve", "tensor_tensor", out=tb[:], in0=xT[:, c, blk], in1=rs_[:], op=ALU.mult)
                if c % 4 == 3:
                    k.op("act", "activation", out=hinT[:, c, blk], in_=tb[:], func=AF.Identity,
                         scale=der[:, ai, c:c + 1], bias=modv[:, l, b0 + c, r:r + 1])
                else:
                    k.op("pool", "tensor_scalar", out=hinT[:, c, blk], in0=tb[:], scalar1=der[:, ai, c:c + 1],
                         scalar2=modv[:, l, b0 + c, r:r + 1], op0=ALU.mult, op1=ALU.add)

    def spill():
        k.dma("sp", out=xs_d.rearrange("(c p) t -> p c t", p=128), in_=xT[:], wkeys=["xs"])

    def postnorm(src, gi, fuse_next=False, have_src_stats=False):
        for b in range(2):
            if have_src_stats:
                k.op("act", "activation", out=rsb[b][:], in_=SB[b][:], func=AF.Ln, bias=EPS, scale=1.0)
                k.op("act", "activation", out=rsb[b][:], in_=rsb[b][:], func=AF.Exp, scale=-0.5)
            else:
                stats_rstd(src, b)
        k.nbanks = 8
        pbn = [k.bank(), k.bank()] if fuse_next else None
        i = 0
        for c in range(KC):
            for b in range(2):
                blk = slice(b * 512, (b + 1) * 512)
                xs_t = xstage[i % 4]
                k.dma("sp", out=xs_t[:], in_=xs_d[c * 128:(c + 1) * 128, blk], rkeys=["xs"])
                tb = tmpb[i % 3]
                k.op("dve", "scalar_tensor_tensor", out=tb[:], in0=src[:, c, blk], scalar=der[:, gi, c:c + 1],
                     in1=rsb[b][:], op0=ALU.mult, op1=ALU.mult)
                eng_ = "dve" if i % 3 == 0 else "pool"
                k.op(eng_, "tensor_tensor", out=xT[:, c, blk], in0=tb[:], in1=xs_t[:], op=ALU.add)
                if fuse_next:
                    sq = sqb[i % 2]
                    k.op("act", "activation", out=sq[:], in_=xT[:, c, blk], func=AF.Square)
                    k.op("pe", "matmul", out=pbn[b][:], lhsT=onesD[:], rhs=sq[:], start=(c == 0), stop=(c == KC - 1))
                i += 1
        if fuse_next:
            for b in range(2):
                k.op("act", "activation", out=rsb2[b][:], in_=pbn[b][:], func=AF.Ln, bias=EPS, scale=1.0)
                k.op("act", "activation", out=rsb2[b][:], in_=rsb2[b][:], func=AF.Exp, scale=-0.5)

    def linearT(W2d, Kdim, Mdim, src, epilogue, group=512):
        kc_n = Kdim // 128
        for g in range(Mdim // group):
            def load(g=g):
                wb = k.wbuf([128, kc_n, group])
                k.dma("pool", out=wb[:], in_=W2d[:, g * group:(g + 1) * group].rearrange("(k p) m -> p k m", p=128))
                return wb

            def comp(wb, g=g):
                for mc in range(group // 128):
                    for b in range(2):
                        blk = slice(b * 512, (b + 1) * 512)
                        pb = k.bank()
                        for kc in range(kc_n):
                            k.op("pe", "matmul", out=pb[:], lhsT=wb[:, kc, mc * 128:(mc + 1) * 128], rhs=src[:, kc, blk],
                                 start=(kc == 0), stop=(kc == kc_n - 1))
                        epilogue(pb, g * (group // 128) + mc, b)
            k.stage(load, comp)

    def ffn(l, extra=None):
        extra = list(extra or [])
        hT = [k.view(OFF_S + i * 8 * KiB, [128, 4, TOK], BF16) for i in range(2)]
        rl = [k.view(OFF_S + 16 * KiB + i * 2 * KiB, [128, 512], F32) for i in range(2)]
        acc = xT
        for g in range(16):
            def load_u(g=g):
                wb = k.wbuf([128, KC, 512])
                k.dma("pool", out=wb[:], in_=ffn_up[l, :, g * 512:(g + 1) * 512].rearrange("(k p) m -> p k m", p=128))
                return wb

            def comp_u(wb, g=g):
                h = hT[g % 2]
                i = 0
                for mc in range(4):
                    for b in range(2):
                        blk = slice(b * 512, (b + 1) * 512)
                        pb = k.bank()
                        for kc in range(KC):
                            k.op("pe", "matmul", out=pb[:], lhsT=wb[:, kc, mc * 128:(mc + 1) * 128], rhs=hinT[:, kc, blk],
                                 start=(kc == 0), stop=(kc == KC - 1))
                        r_ = rl[i % 2]
                        i += 1
                        k.op("act", "activation", out=r_[:], in_=pb[:], func=AF.Relu)
                        k.op("act", "activation", out=h[:, mc, blk], in_=r_[:], func=AF.Square)

            def load_d(g=g):
                wb = k.wbuf([128, 4, D])
                k.dma("pool", out=wb[:], in_=ffn_dn[l, g * 512:(g + 1) * 512, :].rearrange("(k p) m -> p k m", p=128))
                return wb

            def comp_d(wb, g=g):
                h = hT[g % 2]
                for j in range(KC):
                    for b in range(2):
                        blk = slice(b * 512, (b + 1) * 512)
                        pb = k.bank()
                        for kc in range(4):
                            k.op("pe", "matmul", out=pb[:], lhsT=wb[:, kc, j * 128:(j + 1) * 128], rhs=h[:, kc, blk],
                                 start=(kc == 0), stop=(kc == 3))
                        if g == 0:
                            k.op("act", "activation", out=acc[:, j, blk], in_=pb[:], func=AF.Copy)
                        else:
                            k.op("dve", "tensor_tensor", out=acc[:, j, blk], in0=acc[:, j, blk], in1=pb[:], op=ALU.add)
                            if g == 15:
                                src_stat(j, b, acc[:, j, blk])
            k.stage(load_u, comp_u)
            if extra:
                k.stage(*extra.pop(0))
            k.stage(load_d, comp_d)
            if extra and g % 2 == 1:
                k.stage(*extra.pop(0))
        for st_ in extra:
            k.stage(*st_)

    def fnet(r, seqs):
        T = seqs[0][1]
        ntl = T // 128
        AB = k.view(OFF_X, [128, 8, 8, 512], BF16)
        CS = k.view(OFF_S, [128, 2, 512], BF16)
        DFT = k.view(OFF_S + 2 * KiB, [128, ntl, 2 * T], BF16)
        mixT = hinT
        scale = 1.0 / math.sqrt(T * 256.0)

        def s0(_):
            k.dma("pool", out=CS[:], in_=c_cs256.rearrange("(k p) m -> p k m", p=128))
            src = c_dft1024 if T == 1024 else c_dft256
            k.dma("pool", out=DFT[:], in_=src.rearrange("(k p) m -> p k m", p=128))
            for ti in range(8):
                for g in range(8):
                    pb = k.bank()
                    for kc in range(2):
                        k.op("pe", "matmul", out=pb[:], lhsT=hinT[:, g * 2 + kc, ti * 128:(ti + 1) * 128], rhs=CS[:, kc, :],
                             start=(kc == 0), stop=(kc == 1))
                    if (ti + g) % 2 == 0:
                        k.op("act", "activation", out=AB[:, ti, g, :], in_=pb[:], func=AF.Copy)
                    else:
                        k.op("dve", "tensor_copy", out=AB[:, ti, g, :], in_=pb[:])
            for (t0, TT) in seqs:
                tl0 = t0 // 128
                nblk = max(1, TT // 512)
                bw = min(TT, 512)
                for fc in range(KC):
                    g, half = fc // 2, fc % 2
                    for nb in range(nblk):
                        pb = k.bank()
                        n_mm = 2 * ntl
                        i = 0
                        for tt in range(ntl):
                            for cs in range(2):
                                k.op("pe", "matmul", out=pb[:, 0:bw],
                                     lhsT=AB[:, tl0 + tt, g, cs * 256 + half * 128: cs * 256 + half * 128 + 128],
                                     rhs=DFT[:, tt, cs * TT + nb * bw: cs * TT + nb * bw + bw],
                                     start=(i == 0), stop=(i == n_mm - 1))
                                i += 1
                        k.op("act", "activation", out=mixT[:, fc, t0 + nb * bw: t0 + nb * bw + bw], in_=pb[:, 0:bw],
                             func=AF.Identity, scale=scale)
        k.stage(None, s0)
        fb = fbias
        k.stage(None, lambda _: k.dma("sp", out=fb[:], in_=fn_b))

        def epi(pb, mc, b):
            blk = slice(b * 512, (b + 1) * 512)
            k.op("act", "activation", out=xT[:, mc, blk], in_=pb[:], func=AF.Identity, bias=fb[:, mc:mc + 1])
            src_stat(mc, b, xT[:, mc, blk])
        linearT(fn_wout[0], D, D, mixT, epi)
        return xT

    def rglru(r, seqs):
        yT = k.view(OFF_S, [128, KC, TOK], BF16)
        so = [OFF_S + 32 * KiB]

        def salloc(shape, dt):
            n = 1
            for s in shape[1:]:
                n *= s
            v = k.view(so[0], shape, dt)
            so[0] += (n * _esz(dt) + 31) // 32 * 32
            assert so[0] <= OFF_W
            return v
        cw = salloc([128, 4, 16], F32)
        cb = salloc([128, 16], F32)
        gb_ = salloc([128, 4, 16], F32)
        lam = salloc([128, 2, 16], F32)
        c8 = salloc([128, 2, 16], F32)
        c16 = salloc([128, 2, 16], F32)
        h0 = salloc([128, 2, 16], F32)
        hout = salloc([128, 4, 2, 16], F32)
        xo = [OFF_X]

        def xalloc(shape, dt):
            n = 1
            for s in shape[1:]:
                n *= s
            v = k.view(xo[0], shape, dt)
            xo[0] += (n * _esz(dt) + 31) // 32 * 32
            assert xo[0] <= OFF_H
            return v
        gbr = xalloc([128, 4, TOK], BF16)
        xc = xalloc([128, 4, TOK], F32)
        xcb = xalloc([128, 4, TOK], BF16)
        xpre = xalloc([128, TOK], F32)
        gw = xalloc([128, 2, 4, 2, 256], BF16)
        rt1 = xalloc([128, TOK], F32)
        it1 = xalloc([128, TOK], F32)
        st1 = xalloc([128, TOK], F32)
        rt = [rt1, salloc([128, TOK], F32)]
        it = [it1, salloc([128, TOK], F32)]
        st_ = [st1, salloc([128, TOK], F32)]
        hs = [xalloc([128, TOK], F32) for _ in range(2)]

        def s0(_):
            k.dma("sp", out=cw[:], in_=lru_cw.rearrange("p (k c) -> p k c", k=4))
            k.dma("sp", out=cb[:], in_=lru_cb)
            k.dma("sp", out=gb_[:], in_=lru_gb.rearrange("p (k c) -> p k c", k=4))
            k.dma("sp", out=lam[:], in_=lru_lam.rearrange("p (k c) -> p k c", k=2))
            if r == 0:
                k.dma("sp", out=h0[:], in_=sth_d.rearrange("d p c -> p d c"))
            k.op("act", "activation", out=c8[:], in_=lam[:], func=AF.Exp, scale=-1.0)
            k.op("act", "activation", out=c8[:], in_=c8[:], func=AF.Ln, bias=1.0, scale=1.0)
            k.op("dve", "tensor_scalar", out=c16[:], in0=c8[:], scalar1=-16.0, scalar2=None, op0=ALU.mult)
            k.op("dve", "tensor_scalar", out=c8[:], in0=c8[:], scalar1=-8.0, scalar2=None, op0=ALU.mult)
        k.stage(None, s0)

        for gq in range(4):
            def load_g(gq=gq):
                wb = k.wbuf([128, KC, 512])
                k.dma("pool", out=wb[:], in_=lru_win[0, :, gq * 512:(gq + 1) * 512].rearrange("(k p) m -> p k m", p=128))
                return wb

            def comp_g(wb, gq=gq):
                for mc in range(4):
                    for b in range(2):
                        blk = slice(b * 512, (b + 1) * 512)
                        pb = k.bank()
                        for kc in range(KC):
                            k.op("pe", "matmul", out=pb[:], lhsT=wb[:, kc, mc * 128:(mc + 1) * 128], rhs=hinT[:, kc, blk],
                                 start=(kc == 0), stop=(kc == KC - 1))
                        k.op("act", "activation", out=gbr[:, mc, blk], in_=pb[:], func=AF.Gelu_apprx_tanh)

            def load_x(gq=gq):
                wb = k.wbuf([128, KC, 512])
                k.dma("pool", out=wb[:], in_=lru_win[0, :, D + gq * 512: D + (gq + 1) * 512].rearrange("(k p) m -> p k m", p=128))
                return wb

            def comp_x(wb, gq=gq):
                for bl in range(2):
                    n = gq * 2 + bl
                    for d in range(2):
                        for g_ in range(2):
                            k.dma("pool", out=gw[:, bl, d * 2 + g_, :, :],
                                  in_=lru_gw[0, d, g_, n].rearrange("(k p) j -> p k j", p=128))
                for mc in range(4):
                    ch = gq * 4 + mc
                    for b in range(2):
                        blk = slice(b * 512, (b + 1) * 512)
                        pb = k.bank()
                        for kc in range(KC):
                            k.op("pe", "matmul", out=pb[:], lhsT=wb[:, kc, mc * 128:(mc + 1) * 128], rhs=hinT[:, kc, blk],
                                 start=(kc == 0), stop=(kc == KC - 1))
                        k.op("act", "activation", out=xpre[:, blk], in_=pb[:], func=AF.Copy)
                    k.op("dve", "tensor_scalar", out=xc[:, mc, :], in0=xpre[:], scalar1=cw[:, 1, ch:ch + 1],
                         scalar2=cb[:, ch:ch + 1], op0=ALU.mult, op1=ALU.add)
                    for (t0, TT) in seqs:
                        e = t0 + TT
                        k.op("dve", "scalar_tensor_tensor", out=xc[:, mc, t0 + 1:e], in0=xpre[:, t0:e - 1],
                             scalar=cw[:, 0, ch:ch + 1], in1=xc[:, mc, t0 + 1:e], op0=ALU.mult, op1=ALU.add)
                        k.op("dve", "scalar_tensor_tensor", out=xc[:, mc, t0:e - 1], in0=xpre[:, t0 + 1:e],
                             scalar=cw[:, 2, ch:ch + 1], in1=xc[:, mc, t0:e - 1], op0=ALU.mult, op1=ALU.add)
                        k.op("dve", "scalar_tensor_tensor", out=xc[:, mc, t0:e - 2], in0=xpre[:, t0 + 2:e],
                             scalar=cw[:, 3, ch:ch + 1], in1=xc[:, mc, t0:e - 2], op0=ALU.mult, op1=ALU.add)
                    k.op("act", "activation", out=xcb[:, mc, :], in_=xc[:, mc, :], func=AF.Copy)
                for mc in range(4):
                    ch = gq * 4 + mc
                    bl, jc = mc // 2, mc % 2
                    for d in range(2):
                        for g_ in range(2):
                            dst = rt[d] if g_ == 0 else it[d]
                            for b in range(2):
                                blk = slice(b * 512, (b + 1) * 512)
                                pb = k.bank()
                                for kc in range(2):
                                    k.op("pe", "matmul", out=pb[:], lhsT=gw[:, bl, d * 2 + g_, kc, jc * 128:(jc + 1) * 128],
                                         rhs=xcb[:, bl * 2 + kc, blk], start=(kc == 0), stop=(kc == 1))
                                k.op("act", "activation", out=dst[:, blk], in_=pb[:], func=AF.Sigmoid,
                                     bias=gb_[:, d * 2 + g_, ch:ch + 1])
                    for d in range(2):
                        k.op("act", "activation", out=st_[d][:], in_=rt[d][:], func=AF.Exp, scale=c16[:, d, ch:ch + 1])
                        k.op("act", "activation", out=rt[d][:], in_=rt[d][:], func=AF.Exp, scale=c8[:, d, ch:ch + 1])
                    for d in range(2):
                        k.op("act", "activation", out=st_[d][:], in_=st_[d][:], func=AF.Sqrt, scale=-1.0, bias=1.0)
                    for d in range(2):
                        k.op("dve", "tensor_tensor", out=it[d][:], in0=it[d][:], in1=st_[d][:], op=ALU.mult)
                        k.op("dve", "tensor_tensor", out=it[d][:], in0=it[d][:], in1=xc[:, mc, :], op=ALU.mult)
                        for si, (t0, TT) in enumerate(seqs):
                            sl = slice(t0, t0 + TT)
                            init = h0[:, d, ch:ch + 1] if r == 0 else 0.0
                            if d == 0:
                                k.op("dve", "tensor_tensor_scan", out=hs[d][:, sl], data0=rt[d][:, sl], data1=it[d][:, sl],
                                     initial=init, op0=ALU.mult, op1=ALU.add)
                            else:
                                k.op("dve", "tensor_tensor_scan", out=hs[d][:, sl][:, ::-1], data0=rt[d][:, sl][:, ::-1],
                                     data1=it[d][:, sl][:, ::-1], initial=init, op0=ALU.mult, op1=ALU.add)
                            if r == 1:
                                idx = t0 + TT - 1 if d == 0 else t0
                                k.op("pool", "tensor_copy", out=hout[:, si, d, ch:ch + 1], in_=hs[d][:, idx:idx + 1])
                    k.op("dve", "tensor_tensor", out=hs[0][:], in0=hs[0][:], in1=hs[1][:], op=ALU.add)
                    k.op("dve", "tensor_tensor", out=yT[:, ch, :], in0=hs[0][:], in1=gbr[:, mc, :], op=ALU.mult)
            k.stage(load_g, comp_g)
            k.stage(load_x, comp_x)

        if r == 1:
            k.stage(None, lambda _: k.dma("sp", out=newh_o.rearrange("s d p c -> p s d c"), in_=hout[:]))

        def epi(pb, mc, b):
            blk = slice(b * 512, (b + 1) * 512)
            k.op("act", "activation", out=xT[:, mc, blk], in_=pb[:], func=AF.Copy)
            src_stat(mc, b, xT[:, mc, blk])
        linearT(lru_wout[0], D, D, yT, epi)
        return xT

    def mlstm(j, r, seqs):
        W = ml_win[j]
        vt = k.view(OFF_X, [128, 4, 8, 512], BF16)
        hn = k.view(OFF_X + 32 * KiB, [128, 4, 8, 512], BF16)
        yT = k.view(OFF_X, [128, KC, TOK], BF16)
        so = [OFF_S]

        def salloc(shape, dt):
            n = 1
            for s in shape[1:]:
                n *= s
            nb = n * _esz(dt)
            if nb >= 256:
                so[0] = (so[0] + 255) // 256 * 256
            v = k.view(so[0], shape, dt)
            so[0] += (nb + 31) // 32 * 32
            assert so[0] <= OFF_W, so[0]
            return v
        qT = salloc([128, 2, TOK], BF16)
        kT = salloc([128, 2, TOK], BF16)
        hsum = salloc([128, 8, 512], F32)
        Cst = [salloc([128, 2, 512], F32) for _ in range(2)]
        Cbf = salloc([128, 2, 512], BF16)
        Cbf_b = k.view(OFF_X + 62 * KiB, [128, 2, 512], BF16)
        Cbf2 = [Cbf, Cbf_b]
        nst = [salloc([128, 2], F32) for _ in range(2)]
        nbf = salloc([128, 2], BF16)
        nbf2 = [nbf, salloc([128, 2], BF16)]
        MB = salloc([128, TOK], F32)
        cols = salloc([128, 3, 2, 8, 4], F32)
        tn = salloc([128, 512], F32)
        Dm2 = [salloc([128, 128], F32) for _ in range(2)]
        PS2 = [salloc([128, 128], BF16) for _ in range(2)]
        kw2 = [salloc([128, 256], BF16) for _ in range(2)]
        small2 = [salloc([128, 16], F32) for _ in range(2)]
        m0bc = salloc([128, 8], F32)
        maskb = salloc([128, 2, 128], F32)
        ngc = salloc([128, 16], F32)
        ro = [OFF_X + 32 * KiB]

        def ralloc():
            v = k.view(ro[0], [4, TOK], F32)
            ro[0] += 4 * KiB
            assert ro[0] <= OFF_H
            return v
        G = [ralloc() for _ in range(4)]
        Fn = [ralloc() for _ in range(2)]
        Mr = [salloc([4, TOK], F32) for _ in range(2)]
        bg = salloc([4, 4], F32)
        m0t = salloc([4, 2], F32)
        mo = salloc([4, 4, 2], F32)
        sel4 = salloc([4, 4, 128], F32)
        id4 = salloc([4, 4], F32)
        wg = salloc([128, KC, 16], BF16)

        def s_init(_):
            k.dma("sp", out=bg[:], in_=ml_bg[j])
            k.dma("sp", out=sel4[:], in_=c_sel4.rearrange("h (a m) -> h a m", a=4))
            k.dma("sp", out=id4[:], in_=c_id4)
            k.dma("sp", out=maskb[:], in_=c_mask.rearrange("d s t -> s d t"))
            k.dma("sp", out=ngc[:], in_=ml_ng[j])
            k.dma("pool", out=wg[:], in_=W[:, 6144:6160].rearrange("(k p) m -> p k m", p=128))
            if r == 0:
                k.dma("sp", out=m0t[:], in_=stmT_d[j])
                for d in range(2):
                    for h in range(4):
                        k.dma("sp", out=m0bc[:, d * 4 + h: d * 4 + h + 1], in_=stm_d[j, d, h:h + 1].partition_broadcast(128))
            else:
                k.op("dve", "memset", ap=m0bc[:], constant=0.0)
            for cq in range(4):
                for b in range(2):
                    blk = slice(b * 512, (b + 1) * 512)
                    pb = k.bank()
                    for kc in range(KC):
                        k.op("pe", "matmul", out=pb[0:4, :], lhsT=wg[:, kc, cq * 4:(cq + 1) * 4], rhs=hinT[:, kc, blk],
                             start=(kc == 0), stop=(kc == KC - 1))
                    k.op("act", "activation", out=G[cq][:, blk], in_=pb[0:4, :], func=AF.Identity, bias=bg[:, cq:cq + 1])
            for d in range(2):
                li, lf = G[d * 2], G[d * 2 + 1]
                k.op("act", "activation", out=lf[:], in_=lf[:], func=AF.Exp, scale=-1.0)
                k.op("act", "activation", out=lf[:], in_=lf[:], func=AF.Ln, bias=1.0, scale=1.0)
                k.op("dve", "tensor_scalar", out=lf[:], in0=lf[:], scalar1=0.5, scalar2=None, op0=ALU.mult)
                for si, (t0, TT) in enumerate(seqs):
                    sl = slice(t0, t0 + TT)

                    def dv(ap):
                        return ap[:, sl] if d == 0 else ap[:, sl][:, ::-1]
                    k.op("dve", "tensor_tensor_scan", out=dv(Fn[d]), data0=dv(lf), data1=dv(lf), initial=0.0,
                         op0=ALU.add, op1=ALU.add)
                k.op("dve", "tensor_tensor", out=li[:], in0=li[:], in1=Fn[d][:], op=ALU.add)
                for si, (t0, TT) in enumerate(seqs):
                    sl = slice(t0, t0 + TT)

                    def dv(ap):
                        return ap[:, sl] if d == 0 else ap[:, sl][:, ::-1]
                    init = m0t[:, d:d + 1] if r == 0 else 0.0
                    k.op("dve", "tensor_tensor_scan", out=dv(Mr[d]), data0=dv(li), data1=dv(li), initial=init,
                         op0=ALU.max, op1=ALU.max)
                    if r == 1:
                        idx = t0 + TT - 1 if d == 0 else t0
                        k.op("dve", "tensor_tensor", out=mo[:, si, d:d + 1], in0=Mr[d][:, idx:idx + 1],
                             in1=Fn[d][:, idx:idx + 1], op=ALU.subtract)
                k.op("dve", "tensor_tensor", out=lf[:], in0=Fn[d][:], in1=Mr[d][:], op=ALU.subtract)
                k.op("act", "activation", out=lf[:], in_=lf[:], func=AF.Exp)
            pb = k.bank()
            for qi in range(3):
                for d in range(2):
                    src = (G[d * 2], Mr[d], G[d * 2 + 1])[qi]
                    for ti in range(8):
                        o = ((qi * 2 + d) * 8 + ti) * 4
                        k.op("pe", "matmul", out=pb[:, o:o + 4], lhsT=src[:, ti * 128:(ti + 1) * 128], rhs=id4[:],
                             start=True, stop=True)
            k.op("act", "activation", out=cols[:].rearrange("p a d t h -> p (a d t h)"), in_=pb[:, 0:192], func=AF.Copy)
            if r == 1:
                k.dma("sp", out=newm_o[j], in_=mo[:].rearrange("h s d -> h (s d)"))
        k.stage(None, s_init)
        if cfg.get('ml_stop', 99) <= 1:
            return outT_alt

        for h in range(4):
            def load_v(h=h):
                wb = k.wbuf([128, KC, 512])
                k.dma("pool", out=wb[:], in_=W[:, 2048 + h * 512: 2048 + (h + 1) * 512].rearrange("(k p) m -> p k m", p=128))
                return wb

            def comp_v(wb, h=h):
                for ti in range(8):
                    pb = k.bank()
                    for kc in range(KC):
                        k.op("pe", "matmul", out=pb[:], lhsT=hinT[:, kc, ti * 128:(ti + 1) * 128], rhs=wb[:, kc, :],
                             start=(kc == 0), stop=(kc == KC - 1))
                    if ti % 2 == 0:
                        k.op("act", "activation", out=vt[:, h, ti, :], in_=pb[:], func=AF.Copy)
                    else:
                        k.op("dve", "tensor_copy", out=vt[:, h, ti, :], in_=pb[:])
            k.stage(load_v, comp_v)

        if cfg.get('ml_stop', 99) <= 2:
            return outT_alt
        mb_off = [kk for kk, vv in k.views.items() if vv is MB][0][0]
        cst_off = [kk for kk, vv in k.views.items() if vv is Cst[0]][0][0]
        RB = [k.banks[5], k.banks[6], k.banks[7]]
        rot = [0]
        nheads = cfg.get('ml_heads', 4)

        def o_tile(wb, h, ti, loc, n):
            sg = k.view(loc + 2 * KiB, [128, 512], F32)
            yb = k.view(loc + (ti % 2) * KiB, [128, 512], BF16)
            pb = k.bank()
            for kc in range(KC):
                k.op("pe", "matmul", out=pb[:], lhsT=hinT[:, kc, ti * 128:(ti + 1) * 128], rhs=wb[:, kc, :],
                     start=(kc == 0), stop=(kc == KC - 1))
            k.op("act", "activation", out=sg[:], in_=pb[:], func=AF.Sigmoid)
            k.op("dve", "tensor_tensor", out=yb[:], in0=sg[:], in1=hn[:, h, ti, :], op=ALU.mult)
            pbt = k.bank_bf()
            for c in range(4):
                k.op("pe", "transpose", out=pbt[:, c * 128:(c + 1) * 128], in_=yb[:, c * 128:(c + 1) * 128], identity=ident[:])
            src = pbt[:, 0:512].rearrange("p (c t) -> p c t", c=4)
            dst = yT[:, h * 4:(h + 1) * 4, ti * 128:(ti + 1) * 128]
            if n % 2 == 0:
                k.op("act", "activation", out=dst, in_=src, func=AF.Copy)
            else:
                k.op("dve", "tensor_copy", out=dst, in_=src)

        def recur(h, wb_o):
            steps = []
            for d in range(2):
                for si, (t0, TT) in enumerate(seqs):
                    nt = TT // 128
                    tl0 = t0 // 128
                    order = list(range(nt)) if d == 0 else list(range(nt - 1, -1, -1))
                    for oi, tl in enumerate(order):
                        steps.append(dict(d=d, si=si, ti=tl0 + tl, first=(oi == 0), last=(oi == nt - 1),
                                          newd=(si == 0 and oi == 0)))
            nsteps = len(steps)
            cur = {}

            def pre(g):
                s_ = steps[g]
                d, ti = s_['d'], s_['ti']
                par = g % 2
                sm = small2[par]
                if s_['newd']:
                    for b in range(2):
                        blk = slice(b * 512, (b + 1) * 512)
                        pb = k.bank()
                        k.op("pe", "matmul", out=pb[:], lhsT=sel4[:, h, :], rhs=Mr[d][:, blk], start=True, stop=True)
                        k.op("act", "activation", out=MB[:, blk], in_=pb[:], func=AF.Copy)
                tsl = slice(ti * 128, (ti + 1) * 128)
                have_state = (r == 0) or (not s_['first'])
                need_update = (r == 1) or (not s_['last'])
                acol = cols[:, 0, d, ti, h:h + 1]
                mcol = cols[:, 1, d, ti, h:h + 1]
                if s_['first']:
                    mprev = m0bc[:, d * 4 + h: d * 4 + h + 1]
                else:
                    pi = (ti * 128 - 1) if d == 0 else ((ti + 1) * 128)
                    mprev = MB[:, pi:pi + 1]
                ei = ((ti + 1) * 128 - 1) if d == 0 else (ti * 128)
                mend = MB[:, ei:ei + 1]
                pss = k.bank()
                for c in range(2):
                    k.op("pe", "matmul", out=pss[:, 0:128], lhsT=kT[:, c, tsl], rhs=qT[:, c, tsl], start=(c == 0), stop=(c == 1))
                Dm, PS, kw = Dm2[par], PS2[par], kw2[par]
                k.op("dve", "scalar_tensor_tensor", out=Dm[:], in0=MB[:, tsl], scalar=acol, in1=maskb[:, d, :],
                     op0=ALU.subtract, op1=ALU.max)
                k.op("act", "activation", out=Dm[:], in_=Dm[:], func=AF.Exp, scale=-1.0)
                k.op("dve", "tensor_tensor", out=PS[:], in0=Dm[:], in1=pss[:, 0:128], op=ALU.mult)
                psn = RB[par]
                psd = RB[2][:, par * 4: par * 4 + 2]
                k.op("pe", "matmul", out=psn[:], lhsT=PS[:], rhs=vt[:, h, ti, :], start=True, stop=True)
                k.op("pe", "matmul", out=psd[:, 0:1], lhsT=PS[:], rhs=ones1[:, 0:1], start=True, stop=True)
                if have_state:
                    k.op("act", "activation", out=sm[:, 0:1], in_=mcol, func=AF.Exp, scale=-1.0, bias=mprev)
                if need_update:
                    k.op("act", "activation", out=sm[:, 8:9], in_=mend, func=AF.Exp, scale=-1.0, bias=acol)
                    k.op("act", "activation", out=sm[:, 9:10], in_=mend, func=AF.Exp, scale=-1.0, bias=mprev)
                    pbt = k.bank_bf()
                    for c in range(2):
                        k.op("pe", "transpose", out=pbt[:, c * 128:(c + 1) * 128], in_=kT[:, c, tsl], identity=ident[:])
                    k.op("act", "activation", out=kw[:], in_=pbt[:, 0:256], func=AF.Identity, scale=sm[:, 8:9])

            def post(g):
                s_ = steps[g]
                d, ti, si = s_['d'], s_['ti'], s_['si']
                par = g % 2
                sm = small2[par]
                kw = kw2[par]
                tsl = slice(ti * 128, (ti + 1) * 128)
                first, last = s_['first'], s_['last']
                have_state = (r == 0) or (not first)
                need_update = (r == 1) or (not last)
                emcol = cols[:, 2, d, ti, h:h + 1]
                psn = RB[par]
                psd = RB[2][:, par * 4: par * 4 + 2]
                if first:
                    cur['C'] = Cst[rot[0] % 2]
                    cur['n'] = nst[rot[0] % 2]
                    rot[0] += 1
                    cur.setdefault('v', 0)
                    C, nn = cur['C'], cur['n']
                    if r == 0:
                        k.dma("sp", out=C[:], in_=stC_d[j, d, h].rearrange("(c p) v -> p c v", p=128))
                        k.dma("sp", out=nn[:], in_=stn_d[j, d, h])
                        k.op("act", "activation", out=Cbf2[cur['v']][:], in_=C[:], func=AF.Copy)
                        k.op("act", "activation", out=nbf2[cur['v']][:], in_=nn[:], func=AF.Copy)
                    else:
                        k.op("dve", "memset", ap=C[:], constant=0.0)
                        k.op("dve", "memset", ap=nn[:], constant=0.0)
                C, nn = cur['C'], cur['n']
                v = cur['v']
                if need_update:
                    psc = [k.bank(), k.bank()]
                    for c in range(2):
                        k.op("pe", "matmul", out=psc[c][:], lhsT=kw[:, c * 128:(c + 1) * 128], rhs=vt[:, h, ti, :],
                             start=True, stop=True)
                    psdn = RB[2][:, 8:10]
                    for c in range(2):
                        k.op("pe", "matmul", out=psdn[:, c:c + 1], lhsT=kw[:, c * 128:(c + 1) * 128], rhs=ones1[:, 0:1],
                             start=True, stop=True)
                if have_state:
                    psi = k.bank()
                    for c in range(2):
                        k.op("pe", "matmul", out=psi[:], lhsT=qT[:, c, tsl], rhs=Cbf2[v][:, c, :], start=(c == 0), stop=(c == 1))
                    for c in range(2):
                        k.op("pe", "matmul", out=psd[:, 1:2], lhsT=qT[:, c, tsl], rhs=nbf2[v][:, c:c + 1], start=(c == 0), stop=(c == 1))
                if need_update:
                    for c in range(2):
                        k.op("dve", "scalar_tensor_tensor", out=C[:, c, :], in0=C[:, c, :], scalar=sm[:, 9:10],
                             in1=psc[c][:], op0=ALU.mult, op1=ALU.add)
                    k.op("dve", "scalar_tensor_tensor", out=nn[:], in0=nn[:], scalar=sm[:, 9:10],
                         in1=psdn[:, 0:2], op0=ALU.mult, op1=ALU.add)
                    if not last:
                        k.op("act", "activation", out=Cbf2[1 - v][:], in_=C[:], func=AF.Copy)
                        k.op("act", "activation", out=nbf2[1 - v][:], in_=nn[:], func=AF.Copy)
                        cur['v'] = 1 - v
                if have_state:
                    k.op("act", "activation", out=sm[:, 1:3], in_=psd[:, 0:2], func=AF.Copy)
                    k.op("dve", "scalar_tensor_tensor", out=sm[:, 3:4], in0=sm[:, 2:3], scalar=sm[:, 0:1],
                         in1=sm[:, 1:2], op0=ALU.mult, op1=ALU.add)
                else:
                    k.op("act", "activation", out=sm[:, 3:4], in_=psd[:, 0:1], func=AF.Copy)
                k.op("dve", "scalar_tensor_tensor", out=sm[:, 4:5], in0=sm[:, 3:4], scalar=-1.0, in1=sm[:, 3:4], op0=ALU.mult, op1=ALU.max)
                k.op("dve", "tensor_tensor", out=sm[:, 4:5], in0=sm[:, 4:5], in1=emcol, op=ALU.max)
                k.op("dve", "reciprocal", out=sm[:, 5:6], in_=sm[:, 4:5])
                if have_state:
                    k.op("dve", "tensor_tensor", out=sm[:, 6:7], in0=sm[:, 5:6], in1=sm[:, 0:1], op=ALU.mult)
                    k.op("act", "activation", out=tn[:], in_=psn[:], func=AF.Identity, scale=sm[:, 5:6])
                    if d == 0:
                        k.op("dve", "scalar_tensor_tensor", out=hsum[:, ti, :], in0=psi[:], scalar=sm[:, 6:7],
                             in1=tn[:], op0=ALU.mult, op1=ALU.add)
                    else:
                        k.op("dve", "scalar_tensor_tensor", out=tn[:], in0=psi[:], scalar=sm[:, 6:7],
                             in1=tn[:], op0=ALU.mult, op1=ALU.add)
                        k.op("dve", "tensor_tensor", out=hsum[:, ti, :], in0=hsum[:, ti, :], in1=tn[:], op=ALU.add)
                else:
                    if d == 0:
                        k.op("act", "activation", out=hsum[:, ti, :], in_=psn[:], func=AF.Identity, scale=sm[:, 5:6])
                    else:
                        k.op("dve", "scalar_tensor_tensor", out=hsum[:, ti, :], in0=psn[:], scalar=sm[:, 5:6],
                             in1=hsum[:, ti, :], op0=ALU.mult, op1=ALU.add)
                if last and r == 1:
                    k.dma("sp", out=newC_o[si, j, d, h].rearrange("(c p) v -> p c v", p=128), in_=C[:])
                    k.dma("sp", out=newn_o[si, j, d, h], in_=nn[:])

            k.nbanks = 3
            OB = k.banks[4]
            OT = k.as_bf(k.banks[3])
            loc = OFF_X + 56 * KiB
            sg = k.view(loc + 2 * KiB, [128, 512], F32)

            def o_stage(g):
                if wb_o is None:
                    return
                hh = h - 1
                if g >= 3 and (g - 3) % 2 == 0 and (g - 3) // 2 < 8:
                    n = (g - 3) // 2
                    k.op("act", "activation", out=yT[:, hh * 4:(hh + 1) * 4, n * 128:(n + 1) * 128],
                         in_=OT[:, 0:512].rearrange("p (c t) -> p c t", c=4), func=AF.Copy)
                if g >= 2 and (g - 2) % 2 == 0 and (g - 2) // 2 < 8:
                    n = (g - 2) // 2
                    yb = k.view(loc + (n % 2) * KiB, [128, 512], BF16)
                    for c in range(4):
                        k.op("pe", "transpose", out=OT[:, c * 128:(c + 1) * 128], in_=yb[:, c * 128:(c + 1) * 128], identity=ident[:])
                if g >= 1 and (g - 1) % 2 == 0 and (g - 1) // 2 < 8:
                    n = (g - 1) // 2
                    yb = k.view(loc + (n % 2) * KiB, [128, 512], BF16)
                    k.op("act", "activation", out=sg[:], in_=OB[:], func=AF.Exp, scale=-1.0)
                    k.op("act", "activation", out=sg[:], in_=sg[:], func=AF.Ln, bias=1.0, scale=1.0)
                    k.op("act", "activation", out=sg[:], in_=sg[:], func=AF.Exp, scale=-1.0)
                    k.op("pool", "tensor_tensor", out=yb[:], in0=sg[:], in1=hn[:, hh, n, :], op=ALU.mult)
                if g % 2 == 0 and g // 2 < 8:
                    n = g // 2
                    for kc in range(KC):
                        k.op("pe", "matmul", out=OB[:], lhsT=hinT[:, kc, n * 128:(n + 1) * 128], rhs=wb_o[:, kc, :],
                             start=(kc == 0), stop=(kc == KC - 1))

            pre(0)
            for g in range(nsteps):
                if g + 1 < nsteps:
                    pre(g + 1)
                post(g)
                o_stage(g)
            for g in range(nsteps, nsteps + 4):
                o_stage(g)
            k.nbanks = 8
            if h < 3:
                ngb = k.view(OFF_X + 60 * KiB, [128, 512], F32)
            else:
                ngb = k.view(cst_off, [128, 512], F32)
            k.dma("sp", out=ngb[:], in_=ml_ngrow[j, h * 512:(h + 1) * 512].partition_broadcast(128))
            sm = small2[0]
            for ti in range(8):
                k.op("act", "activation", out=tn[:], in_=hsum[:, ti, :], func=AF.Square, accum_out=sm[:, 10:11])
                k.op("act", "activation", out=sm[:, 11:12], in_=sm[:, 10:11], func=AF.Ln, scale=1.0 / 512.0, bias=EPS)
                k.op("act", "activation", out=sm[:, 12:13], in_=sm[:, 11:12], func=AF.Exp, scale=-0.5)
                k.op("dve", "scalar_tensor_tensor", out=hn[:, h, ti, :], in0=hsum[:, ti, :], scalar=sm[:, 12:13],
                     in1=ngb[:], op0=ALU.mult, op1=ALU.mult)

        def load_o_fn(h):
            def load_o():
                wb = k.wbuf([128, KC, 512])
                k.dma("pool", out=wb[:], in_=W[:, 4096 + h * 512: 4096 + (h + 1) * 512].rearrange("(k p) m -> p k m", p=128))
                return wb
            return load_o

        for h in range(nheads):
            def load_qk(h=h):
                wb = k.wbuf([128, KC, 512])
                k.dma("pool", out=wb[:, :, 0:256], in_=W[:, h * 256:(h + 1) * 256].rearrange("(k p) m -> p k m", p=128))
                k.dma("pool", out=wb[:, :, 256:512], in_=W[:, 1024 + h * 256: 1024 + (h + 1) * 256].rearrange("(k p) m -> p k m", p=128))
                return wb

            def comp_qk(wb, h=h):
                for mc in range(4):
                    for b in range(2):
                        blk = slice(b * 512, (b + 1) * 512)
                        pb = k.bank()
                        for kc in range(KC):
                            k.op("pe", "matmul", out=pb[:], lhsT=wb[:, kc, mc * 128:(mc + 1) * 128], rhs=hinT[:, kc, blk],
                                 start=(kc == 0), stop=(kc == KC - 1))
                        if mc < 2:
                            k.op("act", "activation", out=qT[:, mc, blk], in_=pb[:], func=AF.Copy)
                        else:
                            k.op("act", "activation", out=kT[:, mc - 2, blk], in_=pb[:], func=AF.Identity, scale=1.0 / 16.0)
            k.stage(load_qk, comp_qk)
            if h == 0:
                k.stage(None, lambda _, h=h: recur(h, None))
            else:
                k.stage(load_o_fn(h - 1), lambda wb, h=h: recur(h, wb))

        def comp_o_last(wb):
            k.nbanks = 8
            for ti in range(8):
                o_tile(wb, nheads - 1, ti, mb_off, ti)
        k.stage(load_o_fn(nheads - 1), comp_o_last)

        def epi(pb, mc, b):
            blk = slice(b * 512, (b + 1) * 512)
            k.op("act", "activation", out=outT_alt[:, mc, blk], in_=pb[:], func=AF.Copy)
            src_stat(mc, b, outT_alt[:, mc, blk])
        linearT(ml_wout[j], D, D, yT, epi)
        return outT_alt

    for r in passes:
        seqs = [(0, 1024)] if r == 0 else [(i * 256, 256) for i in range(4)]
        k.stage(None, lambda _, r=r: k.dma("sp", out=xT[:], in_=xT_in[r].rearrange("(c p) t -> p c t", p=128)))
        for l in range(nlayers):
            def pre1(_, l=l, r=r):
                derive(l, r)
                prenorm(l, r, 0, have_stats=(l > 0))
                spill()
            k.stage(None, pre1)
            kind, j = l % 3, l // 3
            if cfg.get('skip_mixer'):
                k.stage(None, lambda _: None)
                src = xT
            elif kind == 0:
                src = mlstm(j, r, seqs)
            elif kind == 1:
                src = fnet(r, seqs)
            else:
                src = rglru(r, seqs)
            last_sub = stop_after_mixer and l == nlayers - 1
            k.stage(None, lambda _, src=src, f=(not last_sub): postnorm(src, 1, fuse_next=f, have_src_stats=not cfg.get('skip_mixer')))
            if last_sub:
                break

            def pre2(_, l=l, r=r):
                prenorm(l, r, 1, have_stats=True)
                spill()
            k.stage(None, pre2)
            ex = None
            if r == passes[0] and l + 1 < nlayers and not cfg.get('skip_mod'):
                ex = mod_stages(l + 1)
            ffn(l, ex)
            k.stage(None, lambda _, f=(l + 1 < nlayers): postnorm(xT, 3, fuse_next=f, have_src_stats=True))
        k.stage(None, lambda _, r=r: k.dma("sp", out=yT_out[r].rearrange("(c p) t -> p c t", p=128), in_=xT[:]))
    k.run(depth=2)
    k.finish()
    return k


def _fm(v):
    s = v.shape
    return np.ascontiguousarray(v.reshape(s[:-1] + (s[-1] // 128, 128)).swapaxes(-1, -2))


def _consts():
    c = {}
    c["c_ident"] = np.eye(128, dtype=np.float32)
    i = np.arange(256)
    ang = 2.0 * np.pi * ((i[:, None] * i[None, :]) % 256) / 256.0
    c["c_cs256"] = np.concatenate([np.cos(ang), np.sin(ang)], axis=1).astype(np.float32)
    for T in (1024, 256):
        t = np.arange(T)
        ang = 2.0 * np.pi * ((t[:, None] * t[None, :]) % T) / float(T)
        c[f"c_dft{T}"] = np.concatenate([np.cos(ang), -np.sin(ang)], axis=1).astype(np.float32)
    s = np.arange(128)
    m0 = np.where(s[:, None] <= s[None, :], 0.0, 30000.0)
    m1 = np.where(s[:, None] >= s[None, :], 0.0, 30000.0)
    c["c_mask"] = np.stack([m0, m1]).astype(np.float32)
    sel = np.zeros((4, 4, 128), np.float32)
    for h in range(4):
        sel[h, h, :] = 1.0
    c["c_sel4"] = sel.reshape(4, 512)
    c["c_id4"] = np.eye(4, dtype=np.float32)
    return c


_CACHE = {}


def _program(cfg=None):
    kx = repr(sorted((cfg or {}).items()))
    if kx not in _CACHE:
        _CACHE[kx] = build_program(cfg)
    return _CACHE[kx]


def make_in_maps(inp, ncores=8, cfg=None):
    f = lambda a: np.ascontiguousarray(np.asarray(a, dtype=np.float32))
    shared = {
        "mod_w": f(inp["mod_w"]),
        "mod_b_fm": f(_fm(f(inp["mod_b"])).transpose(1, 0, 2).reshape(128, 4 * 96)),
        "norm_g_fm": f(_fm(f(inp["norm_g"])).transpose(2, 0, 1, 3).reshape(128, 4 * 4 * 16)),
        "ffn_w_up": f(inp["ffn_w_up"]),
        "ffn_w_down": f(inp["ffn_w_down"]),
        "mlstm_w_in": f(inp["mlstm_w_in"]),
        "mlstm_bg": f(f(inp["mlstm_b_gate"]).reshape(2, 4, 4).transpose(0, 2, 1)),
        "mlstm_ng_fm": f(_fm(f(inp["mlstm_norm_g"]))),
        "mlstm_ng_row": f(inp["mlstm_norm_g"]),
        "mlstm_w_out": f(inp["mlstm_w_out"]),
        "fnet_w_out": f(inp["fnet_w_out"]),
        "fnet_b_fm": f(_fm(f(inp["fnet_b_out"])[0])),
        "lru_w_in": f(inp["lru_w_in"]),
        "lru_cw_fm": f(_fm(f(inp["lru_conv_w"])[0]).transpose(1, 0, 2).reshape(128, 64)),
        "lru_cb_fm": f(_fm(f(inp["lru_conv_b"])[0])),
        "lru_gate_w": f(inp["lru_gate_w"]),
        "lru_gb_fm": f(_fm(f(inp["lru_gate_b"])[0].reshape(4, D)).transpose(1, 0, 2).reshape(128, 64)),
        "lru_lam_fm": f(_fm(f(inp["lru_lambda"])[0]).transpose(1, 0, 2).reshape(128, 32)),
        "lru_w_out": f(inp["lru_w_out"]),
    }
    shared.update(_consts())
    xs, xp = f(inp["x_sample"]), f(inp["x_prompt"])
    cc, cctx = f(inp["c"]), f(inp["c_ctx"])
    sC, sn, sm, sh = f(inp["state_mlstm_C"]), f(inp["state_mlstm_n"]), f(inp["state_mlstm_m"]), f(inp["state_lru_h"])
    maps = []
    for i in range(ncores):
        m = dict(shared)
        m["xT_a"] = f(xs[i].T)
        m["xT_b"] = f(xp[4 * i:4 * i + 4].reshape(TOK, D).T)
        m["cond"] = f(np.stack([_fm(cc[i]), _fm(cctx)]))
        m["stC"] = f(sC[i])
        m["stn"] = f(sn[i].reshape(2, 2, 4, 2, 128).swapaxes(-1, -2))
        m["stm"] = f(sm[i])
        m["stmT"] = f(sm[i].transpose(0, 2, 1))
        m["sth"] = f(_fm(sh[i, 0]))
        if cfg and cfg.get('slim'):
            for kx, shp in _big_shapes(cfg).items():
                m[kx] = _slice_to(m[kx], shp)
        maps.append(m)
    return maps


def gather(results, ncores=8):
    y_p = np.zeros((32, 256, D), np.float32)
    y_s = np.zeros((8, 1024, D), np.float32)
    nC = np.zeros((32, 2, 2, 4, 256, 512), np.float32)
    nn = np.zeros((32, 2, 2, 4, 256), np.float32)
    nm = np.zeros((32, 2, 2, 4), np.float32)
    nh = np.zeros((32, 1, 2, D), np.float32)
    for i in range(ncores):
        r = results[i]
        y_s[i] = r["yT_a"].T
        y_p[4 * i:4 * i + 4] = r["yT_b"].T.reshape(4, 256, D)
        nC[4 * i:4 * i + 4] = r["newC"]
        nn[4 * i:4 * i + 4] = r["newn"].swapaxes(-1, -2).reshape(4, 2, 2, 4, 256)
        nm[4 * i:4 * i + 4] = r["newm"].reshape(2, 4, 4, 2).transpose(2, 0, 3, 1)
        nh[4 * i:4 * i + 4, 0] = r["newh"].swapaxes(-1, -2).reshape(4, 2, D)
    return (y_p, y_s, nC, nn, nm, nh)


def kernel(**inputs):
    nc = _program()
    maps = make_in_maps(inputs)
    res = run_bass_kernel_spmd(nc, maps, core_ids=list(range(8)))
    return gather(res.results)
```

```python
import math
import os
import numpy as np
import concourse.bass as bass
import concourse.mybir as mybir
from concourse.bass_utils import run_bass_kernel_spmd

F32 = mybir.dt.float32
BF16 = mybir.dt.bfloat16
U8 = mybir.dt.uint8
AF = mybir.ActivationFunctionType
ALU = mybir.AluOpType

D = 2048
KC = 16
TOK = 1024
DEPTH = 4
EPS = 1e-6
KiB = 1024
OFF_X = 0
OFF_H = 64 * KiB
OFF_S = 96 * KiB
OFF_W = 152 * KiB
OFF_C = 200 * KiB
ARENA = 212000
WBUF_BYTES = 16 * KiB
NWBUF = 3


def _esz(dt):
    return {F32: 4, BF16: 2, U8: 1}[dt]


_BIG = {
    "mod_w": [4, D, 6 * D], "ffn_w_up": [4, D, 4 * D], "ffn_w_down": [4, 4 * D, D],
    "mlstm_w_in": [2, D, 6160], "mlstm_w_out": [2, D, D], "fnet_w_out": [1, D, D],
    "lru_w_in": [1, D, 2 * D], "lru_gate_w": [1, 2, 2, 8, 256, 256], "lru_w_out": [1, D, D],
    "c_dft1024": [1024, 2048], "stC": [2, 2, 4, 256, 512],
}


def _big_shapes(cfg):
    cfg = cfg or {}
    sh = {kx: list(v) for kx, v in _BIG.items()}
    if not cfg.get("slim"):
        return sh
    nl = cfg.get("nlayers", DEPTH)
    sam = cfg.get("stop_after_mixer", False)
    passes = cfg.get("passes", (0, 1))
    skipmix = cfg.get("skip_mixer", False)
    sh["mod_w"][0] = 1 if cfg.get("skip_mod") else nl
    nffn = nl - 1 if sam else nl
    for n_ in ("ffn_w_up", "ffn_w_down"):
        sh[n_][0] = max(nffn, 1)
        if nffn == 0:
            sh[n_][1] = 128
    nml = 0 if skipmix else (nl + 2) // 3
    for n_ in ("mlstm_w_in", "mlstm_w_out"):
        sh[n_][0] = max(nml, 1)
        if nml == 0:
            sh[n_][1] = 128
    if skipmix or nl < 2:
        sh["fnet_w_out"][1] = 128
    if skipmix or nl < 3:
        sh["lru_w_in"][1] = 128
        sh["lru_w_out"][1] = 128
        sh["lru_gate_w"][3] = 1
    if skipmix or nl < 2 or 0 not in passes:
        sh["c_dft1024"][0] = 128
    if skipmix or 0 not in passes:
        sh["stC"][0] = 1
        sh["stC"][3] = 128
    if cfg.get("skip_mod"):
        sh["mod_w"][1] = 128
    return sh


def _slice_to(a, shape):
    return np.ascontiguousarray(a[tuple(slice(0, s) for s in shape)])


class _Eng:
    def __init__(self, name, h, skip_self=False):
        self.name = name
        self.h = h
        self.sem = None
        self.key = None
        self.cnt = 0
        self.seen = {}
        self.ownkeys = set()
        self.skip_self = skip_self


class _Slot:
    def __init__(self, sem, key):
        self.sem = sem
        self.key = key
        self.val = 0


class KB:
    PAGE = 64
    SEMCAP = 30000

    def __init__(self, marked=None):
        self.nc = bass.Bass("TRN2", target_bir_lowering=False)
        nc = self.nc
        self.marked = marked
        self.used = {}
        self.engkeys = set()
        self.rankmap = None
        if marked is not None:
            self.rankmap = {kx: {idx: i + 1 for i, idx in enumerate(sorted(v))} for kx, v in marked.items()}
        self.arena = nc.alloc_sbuf_tensor("arena", [128, ARENA], U8)
        self.arena_base = nc.lookup_mloc(self.arena).addr
        self.base = {}
        self.pages = {}
        self.nsem = 0
        self.eng = {
            "pe": _Eng("pe", nc.tensor, skip_self=True),
            "act": _Eng("act", nc.scalar),
            "dve": _Eng("dve", nc.vector),
            "pool": _Eng("pool", nc.gpsimd),
            "sp": _Eng("sp", nc.sync),
        }
        for e in ("pe", "act", "dve", "pool"):
            self._new_epoch(self.eng[e])
        self.slots = {
            "sp": [self._mkslot() for _ in range(16)],
            "pool": [self._mkslot() for _ in range(8)],
        }
        self.dma_i = {"sp": 0, "pool": 0}
        self.views = {}
        self.banks = []
        for i in range(8):
            p = nc.alloc_psum_tensor(f"pb{i}", [128, 512], F32)
            ml = nc.lookup_mloc(p)
            self.base[p.name] = ("PS", ml.bank * 2048 + ml.addr)
            self.banks.append(p)
        assert len({self.base[p.name][1] for p in self.banks}) == 8
        self.bank_i = 0
        self.nbanks = 8
        self.wi = 0
        self.stages = []

    def _newsem(self):
        self.nsem += 1
        name = f"s{self.nsem}"
        return self.nc.alloc_semaphore(name), name

    def _mkslot(self):
        s, kx = self._newsem()
        return _Slot(s, kx)

    def _new_epoch(self, e):
        e.sem, e.key = self._newsem()
        e.ownkeys.add(e.key)
        self.engkeys.add(e.key)
        e.cnt = 0

    def _semval(self, kx, val):
        if kx in self.engkeys:
            self.used.setdefault(kx, set()).add(val)
            if self.rankmap is not None:
                return self.rankmap[kx][val]
        return val

    def view(self, off, shape, dt):
        kx = (off, tuple(shape), dt)
        if kx not in self.views:
            h = self.nc.alloc_sbuf_tensor_at(f"v{len(self.views)}", list(shape), dt, offset=self.arena_base + off)
            self.base[h.name] = ("SB", self.arena_base + off)
            self.views[kx] = h
        return self.views[kx]

    def bank(self):
        b = self.banks[self.bank_i % self.nbanks]
        self.bank_i += 1
        return b

    def bank_bf(self):
        return self.as_bf(self.bank())

    def as_bf(self, b):
        hb = b.bitcast(BF16)
        self.base[hb.name] = self.base[b.name]
        return hb

    def wbuf(self, shape, dt=BF16):
        i = self.wi % NWBUF
        self.wi += 1
        return self.view(OFF_W + i * WBUF_BYTES, shape, dt)

    def _rng(self, ap):
        t = ap.tensor
        if t.name not in self.base:
            return None
        space, base = self.base[t.name]
        esz = _esz(ap.dtype)
        shape = list(t.shape)
        pstride = 1
        for s in shape[1:]:
            pstride *= s
        off = ap.offset % pstride
        lo = hi = off
        dims = list(ap.ap)
        for (step, cnt) in dims[1:]:
            ext = step * (cnt - 1)
            if ext < 0:
                lo += ext
            else:
                hi += ext
        return (space, base + lo * esz, base + (hi + 1) * esz)

    def _pagekeys(self, r):
        if r[0] == "K":
            return [r]
        space, lo, hi = r
        P = 2048 if space == "PS" else self.PAGE
        return [(space, pg) for pg in range(lo // P, (hi - 1) // P + 1)]

    def _deps(self, reads, writes):
        deps = {}

        def add(rec):
            if rec is None:
                return
            o = deps.get(rec[1])
            if o is None or o[2] < rec[2]:
                deps[rec[1]] = rec

        for r in reads:
            for pk in self._pagekeys(r):
                st = self.pages.get(pk)
                if st is not None:
                    add(st[0])
        for r in writes:
            for pk in self._pagekeys(r):
                st = self.pages.get(pk)
                if st is not None:
                    add(st[0])
                    for rec in st[1].values():
                        add(rec)
        return deps

    def _commit(self, reads, writes, rec):
        for r in reads:
            for pk in self._pagekeys(r):
                st = self.pages.get(pk)
                if st is None:
                    st = [None, {}]
                    self.pages[pk] = st
                st[1][rec[1]] = rec
        for r in writes:
            for pk in self._pagekeys(r):
                self.pages[pk] = [rec, {}]

    def _wait(self, e, deps):
        for kx, (sem, _, val) in deps.items():
            if kx in e.ownkeys:
                if e.skip_self or kx != e.key or val < e.cnt - 1:
                    continue
            if e.seen.get(kx, 0) >= val:
                continue
            e.h.wait_ge(sem, self._semval(kx, val))
            e.seen[kx] = val

    def op(self, eng, name, **kw):
        e = self.eng[eng]
        if e.cnt >= self.SEMCAP:
            self._new_epoch(e)
        reads, writes = [], []
        for kx, v in kw.items():
            if hasattr(v, "tensor") and hasattr(v, "ap"):
                r = self._rng(v)
                if r is None:
                    continue
                if kx in ("out", "accum_out", "ap"):
                    writes.append(r)
                else:
                    reads.append(r)
        self._wait(e, self._deps(reads, writes))
        ins = getattr(e.h, name)(**kw)
        e.cnt += 1
        if self.marked is None or e.cnt in self.marked.get(e.key, ()):
            ins.then_inc(e.sem, 1)
        self._commit(reads, writes, (e.sem, e.key, e.cnt))
        return ins

    def dma(self, issuer, out, in_, rkeys=(), wkeys=(), **kw):
        e = self.eng[issuer]
        reads = [("K", x) for x in rkeys]
        writes = [("K", x) for x in wkeys]
        r = self._rng(in_)
        if r is not None:
            reads.append(r)
        r = self._rng(out)
        if r is not None:
            writes.append(r)
        self._wait(e, self._deps(reads, writes))
        pool = self.slots[issuer]
        slot = pool[self.dma_i[issuer] % len(pool)]
        self.dma_i[issuer] += 1
        if slot.val > 0 and e.seen.get(slot.key, 0) < slot.val:
            e.h.wait_ge(slot.sem, slot.val)
            e.seen[slot.key] = slot.val
        ins = e.h.dma_start(out=out, in_=in_, **kw)
        slot.val += 16
        ins.then_inc(slot.sem, 16)
        self._commit(reads, writes, (slot.sem, slot.key, slot.val))

    def finish(self):
        e = self.eng["sp"]
        for pool in self.slots.values():
            for s in pool:
                if s.val > 0:
                    e.h.wait_ge(s.sem, s.val)
        for n in ("pe", "act", "dve", "pool"):
            x = self.eng[n]
            if x.cnt > 0:
                e.h.wait_ge(x.sem, self._semval(x.key, x.cnt))

    def stage(self, load, compute):
        self.stages.append((load, compute))

    def run(self, depth=2):
        st = self.stages
        n = len(st)
        load_idx = [i for i in range(n) if st[i][0] is not None]
        hs = {}
        nl = 0
        cdone = 0
        for i in range(n):
            while nl < len(load_idx) and nl < cdone + NWBUF:
                li = load_idx[nl]
                hs[li] = st[li][0]()
                nl += 1
            st[i][1](hs.pop(i, None))
            if st[i][0] is not None:
                cdone += 1
        self.stages = []


def build_program(cfg=None):
    k1 = _emit_program(cfg, None)
    k2 = _emit_program(cfg, k1.used)
    return k2.nc


def _emit_program(cfg=None, marked=None):
    cfg = cfg or {}
    nlayers = cfg.get("nlayers", DEPTH)
    passes = cfg.get("passes", (0, 1))
    stop_after_mixer = cfg.get("stop_after_mixer", False)
    k = KB(marked)
    nc = k.nc

    def din(name, shape):
        return nc.dram_tensor(name, list(shape), F32, kind="ExternalInput").ap()

    bsh = _big_shapes(cfg)

    def dout(name, shape):
        return nc.dram_tensor(name, list(shape), F32, kind="ExternalOutput").ap()

    xT_in = [din("xT_a", [D, TOK]), din("xT_b", [D, TOK])]
    cond_d = din("cond", [2, 128, 16])
    stC_d = din("stC", bsh["stC"])
    stn_d = din("stn", [2, 2, 4, 128, 2])
    stm_d = din("stm", [2, 2, 4])
    stmT_d = din("stmT", [2, 4, 2])
    sth_d = din("sth", [2, 128, 16])
    mod_w = din("mod_w", bsh["mod_w"])
    mod_b = din("mod_b_fm", [128, 4 * 96])
    norm_g = din("norm_g_fm", [128, 4 * 4 * 16])
    ffn_up = din("ffn_w_up", bsh["ffn_w_up"])
    ffn_dn = din("ffn_w_down", bsh["ffn_w_down"])
    ml_win = din("mlstm_w_in", bsh["mlstm_w_in"])
    ml_bg = din("mlstm_bg", [2, 4, 4])
    ml_ng = din("mlstm_ng_fm", [2, 128, 16])
    ml_ngrow = din("mlstm_ng_row", [2, D])
    ml_wout = din("mlstm_w_out", bsh["mlstm_w_out"])
    fn_wout = din("fnet_w_out", bsh["fnet_w_out"])
    fn_b = din("fnet_b_fm", [128, 16])
    lru_win = din("lru_w_in", bsh["lru_w_in"])
    lru_cw = din("lru_cw_fm", [128, 4 * 16])
    lru_cb = din("lru_cb_fm", [128, 16])
    lru_gw = din("lru_gate_w", bsh["lru_gate_w"])
    lru_gb = din("lru_gb_fm", [128, 4 * 16])
    lru_lam = din("lru_lam_fm", [128, 2 * 16])
    lru_wout = din("lru_w_out", bsh["lru_w_out"])
    c_ident = din("c_ident", [128, 128])
    c_cs256 = din("c_cs256", [256, 512])
    c_dft1024 = din("c_dft1024", bsh["c_dft1024"])
    c_dft256 = din("c_dft256", [256, 512])
    c_mask = din("c_mask", [2, 128, 128])
    c_sel4 = din("c_sel4", [4, 4 * 128])
    c_id4 = din("c_id4", [4, 4])

    yT_out = [dout("yT_a", [D, TOK]), dout("yT_b", [D, TOK])]
    newC_o = dout("newC", [4, 2, 2, 4, 256, 512])
    newn_o = dout("newn", [4, 2, 2, 4, 128, 2])
    newm_o = dout("newm", [2, 4, 8])
    newh_o = dout("newh", [4, 2, 128, 16])
    xs_d = nc.dram_tensor("xs_scratch", [D, TOK], F32).ap()

    xT = k.view(OFF_X, [128, KC, TOK], F32)
    hinT = k.view(OFF_H, [128, KC, TOK], BF16)
    outT_alt = k.view(OFF_H, [128, KC, TOK], F32)
    co = [OFF_C]

    def calloc(shape, dt):
        n = 1
        for s in shape[1:]:
            n *= s
        nb = (n * _esz(dt) + 31) // 32 * 32
        v = k.view(co[0], shape, dt)
        co[0] += nb
        assert co[0] <= ARENA
        return v

    modv = calloc([128, 4, 96, 2], F32)
    modb = calloc([128, 4, 96], F32)
    ng = calloc([128, 4, 4, 16], F32)
    der = calloc([128, 4, 16], F32)
    ident = calloc([128, 128], BF16)
    onesD = calloc([128, 128], BF16)
    ones1 = calloc([128, 2], BF16)
    condT = calloc([128, 2, 16], F32)
    scT = calloc([128, 16, 2], BF16)
    fbias = calloc([128, 16], F32)
    NT = OFF_S + 32 * KiB
    sqb = [k.view(NT + i * KiB, [128, 512], BF16) for i in range(2)]
    rsb = [k.view(NT + 2 * KiB + i * 2 * KiB, [128, 512], F32) for i in range(2)]
    rsb2 = [k.view(NT + 6 * KiB + i * 2 * KiB, [128, 512], F32) for i in range(2)]
    tmpb = [k.view(NT + 10 * KiB + i * 2 * KiB, [128, 512], F32) for i in range(3)]
    xstage = [k.view(NT + 16 * KiB + i * 2 * KiB, [128, 512], F32) for i in range(4)]

    k.dma("pool", out=ident[:], in_=c_ident)
    k.dma("sp", out=modb[:], in_=mod_b.rearrange("p (l c) -> p l c", l=4))
    k.dma("sp", out=ng[:], in_=norm_g.rearrange("p (l a c) -> p l a c", l=4, a=4))
    k.dma("sp", out=condT[:], in_=cond_d.rearrange("r p c -> p r c"))
    k.op("dve", "memset", ap=onesD[:], constant=1.0 / D)
    k.op("dve", "memset", ap=ones1[:], constant=1.0)
    for r in range(2):
        k.op("act", "activation", out=scT[:, :, r], in_=condT[:, r, :], func=AF.Silu)


    def mod_stages(l):
        out = []
        for g in range(24):
            def load(l=l, g=g):
                wb = k.wbuf([128, KC, 512])
                k.dma("pool", out=wb[:], in_=mod_w[l, :, g * 512:(g + 1) * 512].rearrange("(k p) m -> p k m", p=128))
                return wb

            def comp(wb, l=l, g=g):
                for mc in range(4):
                    pb = k.bank()
                    for kc in range(KC):
                        k.op("pe", "matmul", out=pb[:, 0:2], lhsT=wb[:, kc, mc * 128:(mc + 1) * 128], rhs=scT[:, kc, :],
                             start=(kc == 0), stop=(kc == KC - 1))
                    c = g * 4 + mc
                    k.op("act", "activation", out=modv[:, l, c, :], in_=pb[:, 0:2], func=AF.Identity,
                         bias=modb[:, l, c:c + 1])
            out.append((load, comp))
        return out

    if not cfg.get('skip_mod'):
        for st_ in mod_stages(0):
            k.stage(*st_)

    def stats_rstd(src, b):
        blk = slice(b * 512, (b + 1) * 512)
        pb = k.bank()
        for c in range(KC):
            sq = sqb[c % 2]
            k.op("act", "activation", out=sq[:], in_=src[:, c, blk], func=AF.Square)
            k.op("pe", "matmul", out=pb[:], lhsT=onesD[:], rhs=sq[:], start=(c == 0), stop=(c == KC - 1))
        k.op("act", "activation", out=rsb[b][:], in_=pb[:], func=AF.Ln, bias=EPS, scale=1.0)
        k.op("act", "activation", out=rsb[b][:], in_=rsb[b][:], func=AF.Exp, scale=-0.5)

    SB = [k.banks[6], k.banks[7]]
    sst = [0]
    spend = []
    sqb4 = [k.view(NT + 10 * KiB + i * KiB, [128, 512], BF16) for i in range(6)]

    def src_stat_flush(keep=0):
        while len(spend) > keep:
            c, b, sq = spend.pop(0)
            k.op("pe", "matmul", out=SB[b][:], lhsT=onesD[:], rhs=sq[:], start=(c == 0), stop=(c == KC - 1))

    def src_stat(c, b, ap):
        if c == 0 and b == 0:
            k.nbanks = 6
        sq = sqb4[sst[0] % 6]
        sst[0] += 1
        k.op("act", "activation", out=sq[:], in_=ap, func=AF.Square)
        spend.append((c, b, sq))
        src_stat_flush(keep=3)

    def derive(l, r):
        mv = modv
        k.op("dve", "scalar_tensor_tensor", out=der[:, 0, :], in0=mv[:, l, 16:32, r], scalar=1.0, in1=ng[:, l, 0, :],
             op0=ALU.add, op1=ALU.mult)
        k.op("dve", "tensor_tensor", out=der[:, 1, :], in0=mv[:, l, 32:48, r], in1=ng[:, l, 1, :], op=ALU.mult)
        k.op("dve", "scalar_tensor_tensor", out=der[:, 2, :], in0=mv[:, l, 64:80, r], scalar=1.0, in1=ng[:, l, 2, :],
             op0=ALU.add, op1=ALU.mult)
        k.op("dve", "tensor_tensor", out=der[:, 3, :], in0=mv[:, l, 80:96, r], in1=ng[:, l, 3, :], op=ALU.mult)

    def prenorm(l, r, which, have_stats=False):
        ai = 0 if which == 0 else 2
        b0 = 0 if which == 0 else 48
        for b in range(2):
            blk = slice(b * 512, (b + 1) * 512)
            if have_stats:
                rs_ = rsb2[b]
            else:
                stats_rstd(xT, b)
                rs_ = rsb[b]
            for c in range(KC):
                tb = tmpb[c % 3]
                k.op("dve", "tensor_tensor", out=tb[:], in0=xT[:, c, blk], in1=rs_[:], op=ALU.mult)
                if c % 4 == 3:
                    k.op("act", "activation", out=hinT[:, c, blk], in_=tb[:], func=AF.Identity,
                         scale=der[:, ai, c:c + 1], bias=modv[:, l, b0 + c, r:r + 1])
                else:
                    k.op("pool", "tensor_scalar", out=hinT[:, c, blk], in0=tb[:], scalar1=der[:, ai, c:c + 1],
                         scalar2=modv[:, l, b0 + c, r:r + 1], op0=ALU.mult, op1=ALU.add)

    def spill():
        k.dma("sp", out=xs_d.rearrange("(c p) t -> p c t", p=128), in_=xT[:], wkeys=["xs"])

    def postnorm(src, gi, fuse_next=False, have_src_stats=False):
        if have_src_stats:
            src_stat_flush(0)
        for b in range(2):
            if have_src_stats:
                k.op("act", "activation", out=rsb[b][:], in_=SB[b][:], func=AF.Ln, bias=EPS, scale=1.0)
                k.op("act", "activation", out=rsb[b][:], in_=rsb[b][:], func=AF.Exp, scale=-0.5)
            else:
                stats_rstd(src, b)
        k.nbanks = 8
        pbn = [k.bank(), k.bank()] if fuse_next else None
        i = 0
        for c in range(KC):
            for b in range(2):
                blk = slice(b * 512, (b + 1) * 512)
                xs_t = xstage[i % 4]
                k.dma("sp", out=xs_t[:], in_=xs_d[c * 128:(c + 1) * 128, blk], rkeys=["xs"])
                tb = tmpb[i % 3]
                k.op("dve", "scalar_tensor_tensor", out=tb[:], in0=src[:, c, blk], scalar=der[:, gi, c:c + 1],
                     in1=rsb[b][:], op0=ALU.mult, op1=ALU.mult)
                eng_ = "dve" if i % 3 == 0 else "pool"
                k.op(eng_, "tensor_tensor", out=xT[:, c, blk], in0=tb[:], in1=xs_t[:], op=ALU.add)
                if fuse_next:
                    sq = sqb[i % 2]
                    k.op("act", "activation", out=sq[:], in_=xT[:, c, blk], func=AF.Square)
                    k.op("pe", "matmul", out=pbn[b][:], lhsT=onesD[:], rhs=sq[:], start=(c == 0), stop=(c == KC - 1))
                i += 1
        if fuse_next:
            for b in range(2):
                k.op("act", "activation", out=rsb2[b][:], in_=pbn[b][:], func=AF.Ln, bias=EPS, scale=1.0)
                k.op("act", "activation", out=rsb2[b][:], in_=rsb2[b][:], func=AF.Exp, scale=-0.5)

    def linearT(W2d, Kdim, Mdim, src, epilogue, group=512):
        kc_n = Kdim // 128
        for g in range(Mdim // group):
            def load(g=g):
                wb = k.wbuf([128, kc_n, group])
                k.dma("pool", out=wb[:], in_=W2d[:, g * group:(g + 1) * group].rearrange("(k p) m -> p k m", p=128))
                return wb

            def comp(wb, g=g):
                for mc in range(group // 128):
                    for b in range(2):
                        blk = slice(b * 512, (b + 1) * 512)
                        pb = k.bank()
                        for kc in range(kc_n):
                            k.op("pe", "matmul", out=pb[:], lhsT=wb[:, kc, mc * 128:(mc + 1) * 128], rhs=src[:, kc, blk],
                                 start=(kc == 0), stop=(kc == kc_n - 1))
                        epilogue(pb, g * (group // 128) + mc, b)
            k.stage(load, comp)

    def ffn(l, extra=None):
        extra = list(extra or [])
        hT = [k.view(OFF_S + i * 8 * KiB, [128, 4, TOK], BF16) for i in range(2)]
        rl = [k.view(OFF_S + 16 * KiB + i * 2 * KiB, [128, 512], F32) for i in range(2)]
        acc = xT
        for g in range(16):
            def load_u(g=g):
                wb = k.wbuf([128, KC, 512])
                k.dma("pool", out=wb[:], in_=ffn_up[l, :, g * 512:(g + 1) * 512].rearrange("(k p) m -> p k m", p=128))
                return wb

            def comp_u(wb, g=g):
                h = hT[g % 2]
                i = 0
                for mc in range(4):
                    for b in range(2):
                        blk = slice(b * 512, (b + 1) * 512)
                        pb = k.bank()
                        for kc in range(KC):
                            k.op("pe", "matmul", out=pb[:], lhsT=wb[:, kc, mc * 128:(mc + 1) * 128], rhs=hinT[:, kc, blk],
                                 start=(kc == 0), stop=(kc == KC - 1))
                        r_ = rl[i % 2]
                        i += 1
                        k.op("act", "activation", out=r_[:], in_=pb[:], func=AF.Relu)
                        k.op("act", "activation", out=h[:, mc, blk], in_=r_[:], func=AF.Square)

            def load_d(g=g):
                wb = k.wbuf([128, 4, D])
                k.dma("pool", out=wb[:], in_=ffn_dn[l, g * 512:(g + 1) * 512, :].rearrange("(k p) m -> p k m", p=128))
                return wb

            def comp_d(wb, g=g):
                h = hT[g % 2]
                for j in range(KC):
                    for b in range(2):
                        blk = slice(b * 512, (b + 1) * 512)
                        pb = k.bank()
                        for kc in range(4):
                            k.op("pe", "matmul", out=pb[:], lhsT=wb[:, kc, j * 128:(j + 1) * 128], rhs=h[:, kc, blk],
                                 start=(kc == 0), stop=(kc == 3))
                        if g == 0:
                            k.op("act", "activation", out=acc[:, j, blk], in_=pb[:], func=AF.Copy)
                        else:
                            k.op("dve", "tensor_tensor", out=acc[:, j, blk], in0=acc[:, j, blk], in1=pb[:], op=ALU.add)
                            if g == 15:
                                src_stat(j, b, acc[:, j, blk])
            k.stage(load_u, comp_u)
            if extra:
                k.stage(*extra.pop(0))
            k.stage(load_d, comp_d)
            if extra and g % 2 == 1:
                k.stage(*extra.pop(0))
        for st_ in extra:
            k.stage(*st_)

    def fnet(r, seqs):
        T = seqs[0][1]
        ntl = T // 128
        AB = k.view(OFF_X, [128, 8, 8, 512], BF16)
        CS = k.view(OFF_S, [128, 2, 512], BF16)
        DFT = k.view(OFF_S + 2 * KiB, [128, ntl, 2 * T], BF16)
        mixT = hinT
        scale = 1.0 / math.sqrt(T * 256.0)

        def s0(_):
            k.dma("pool", out=CS[:], in_=c_cs256.rearrange("(k p) m -> p k m", p=128))
            src = c_dft1024 if T == 1024 else c_dft256
            k.dma("pool", out=DFT[:], in_=src.rearrange("(k p) m -> p k m", p=128))
            for ti in range(8):
                for g in range(8):
                    pb = k.bank()
                    for kc in range(2):
                        k.op("pe", "matmul", out=pb[:], lhsT=hinT[:, g * 2 + kc, ti * 128:(ti + 1) * 128], rhs=CS[:, kc, :],
                             start=(kc == 0), stop=(kc == 1))
                    if (ti + g) % 2 == 0:
                        k.op("act", "activation", out=AB[:, ti, g, :], in_=pb[:], func=AF.Copy)
                    else:
                        k.op("dve", "tensor_copy", out=AB[:, ti, g, :], in_=pb[:])
            for (t0, TT) in seqs:
                tl0 = t0 // 128
                nblk = max(1, TT // 512)
                bw = min(TT, 512)
                for fc in range(KC):
                    g, half = fc // 2, fc % 2
                    for nb in range(nblk):
                        pb = k.bank()
                        n_mm = 2 * ntl
                        i = 0
                        for tt in range(ntl):
                            for cs in range(2):
                                k.op("pe", "matmul", out=pb[:, 0:bw],
                                     lhsT=AB[:, tl0 + tt, g, cs * 256 + half * 128: cs * 256 + half * 128 + 128],
                                     rhs=DFT[:, tt, cs * TT + nb * bw: cs * TT + nb * bw + bw],
                                     start=(i == 0), stop=(i == n_mm - 1))
                                i += 1
                        k.op("act", "activation", out=mixT[:, fc, t0 + nb * bw: t0 + nb * bw + bw], in_=pb[:, 0:bw],
                             func=AF.Identity, scale=scale)
        k.stage(None, s0)
        fb = fbias
        k.stage(None, lambda _: k.dma("sp", out=fb[:], in_=fn_b))

        def epi(pb, mc, b):
            blk = slice(b * 512, (b + 1) * 512)
            k.op("act", "activation", out=xT[:, mc, blk], in_=pb[:], func=AF.Identity, bias=fb[:, mc:mc + 1])
            src_stat(mc, b, xT[:, mc, blk])
        linearT(fn_wout[0], D, D, mixT, epi)
        return xT

    def rglru(r, seqs):
        yT = k.view(OFF_S, [128, KC, TOK], BF16)
        so = [OFF_S + 32 * KiB]

        def salloc(shape, dt):
            n = 1
            for s in shape[1:]:
                n *= s
            v = k.view(so[0], shape, dt)
            so[0] += (n * _esz(dt) + 31) // 32 * 32
            assert so[0] <= OFF_W
            return v
        cw = salloc([128, 4, 16], F32)
        cb = salloc([128, 16], F32)
        gb_ = salloc([128, 4, 16], F32)
        lam = salloc([128, 2, 16], F32)
        c8 = salloc([128, 2, 16], F32)
        c16 = salloc([128, 2, 16], F32)
        h0 = salloc([128, 2, 16], F32)
        hout = salloc([128, 4, 2, 16], F32)
        xo = [OFF_X]

        def xalloc(shape, dt):
            n = 1
            for s in shape[1:]:
                n *= s
            v = k.view(xo[0], shape, dt)
            xo[0] += (n * _esz(dt) + 31) // 32 * 32
            assert xo[0] <= OFF_H
            return v
        gbr = xalloc([128, 4, TOK], BF16)
        xc = xalloc([128, 4, TOK], F32)
        xcb = xalloc([128, 4, TOK], BF16)
        xpre = xalloc([128, TOK], F32)
        gw = xalloc([128, 2, 4, 2, 256], BF16)
        rt1 = xalloc([128, TOK], F32)
        it1 = xalloc([128, TOK], F32)
        st1 = xalloc([128, TOK], F32)
        rt = [rt1, salloc([128, TOK], F32)]
        it = [it1, salloc([128, TOK], F32)]
        st_ = [st1, salloc([128, TOK], F32)]
        hs = [xalloc([128, TOK], F32) for _ in range(2)]

        def s0(_):
            k.dma("sp", out=cw[:], in_=lru_cw.rearrange("p (k c) -> p k c", k=4))
            k.dma("sp", out=cb[:], in_=lru_cb)
            k.dma("sp", out=gb_[:], in_=lru_gb.rearrange("p (k c) -> p k c", k=4))
            k.dma("sp", out=lam[:], in_=lru_lam.rearrange("p (k c) -> p k c", k=2))
            if r == 0:
                k.dma("sp", out=h0[:], in_=sth_d.rearrange("d p c -> p d c"))
            k.op("act", "activation", out=c8[:], in_=lam[:], func=AF.Exp, scale=-1.0)
            k.op("act", "activation", out=c8[:], in_=c8[:], func=AF.Ln, bias=1.0, scale=1.0)
            k.op("dve", "tensor_scalar", out=c16[:], in0=c8[:], scalar1=-16.0, scalar2=None, op0=ALU.mult)
            k.op("dve", "tensor_scalar", out=c8[:], in0=c8[:], scalar1=-8.0, scalar2=None, op0=ALU.mult)
        k.stage(None, s0)

        for gq in range(4):
            def load_g(gq=gq):
                wb = k.wbuf([128, KC, 512])
                k.dma("pool", out=wb[:], in_=lru_win[0, :, gq * 512:(gq + 1) * 512].rearrange("(k p) m -> p k m", p=128))
                return wb

            def comp_g(wb, gq=gq):
                for mc in range(4):
                    for b in range(2):
                        blk = slice(b * 512, (b + 1) * 512)
                        pb = k.bank()
                        for kc in range(KC):
                            k.op("pe", "matmul", out=pb[:], lhsT=wb[:, kc, mc * 128:(mc + 1) * 128], rhs=hinT[:, kc, blk],
                                 start=(kc == 0), stop=(kc == KC - 1))
                        k.op("act", "activation", out=gbr[:, mc, blk], in_=pb[:], func=AF.Gelu_apprx_tanh)

            def load_x(gq=gq):
                wb = k.wbuf([128, KC, 512])
                k.dma("pool", out=wb[:], in_=lru_win[0, :, D + gq * 512: D + (gq + 1) * 512].rearrange("(k p) m -> p k m", p=128))
                return wb

            def comp_x(wb, gq=gq):
                for bl in range(2):
                    n = gq * 2 + bl
                    for d in range(2):
                        for g_ in range(2):
                            k.dma("pool", out=gw[:, bl, d * 2 + g_, :, :],
                                  in_=lru_gw[0, d, g_, n].rearrange("(k p) j -> p k j", p=128))
                for mc in range(4):
                    ch = gq * 4 + mc
                    for b in range(2):
                        blk = slice(b * 512, (b + 1) * 512)
                        pb = k.bank()
                        for kc in range(KC):
                            k.op("pe", "matmul", out=pb[:], lhsT=wb[:, kc, mc * 128:(mc + 1) * 128], rhs=hinT[:, kc, blk],
                                 start=(kc == 0), stop=(kc == KC - 1))
                        k.op("act", "activation", out=xpre[:, blk], in_=pb[:], func=AF.Copy)
                    k.op("dve", "tensor_scalar", out=xc[:, mc, :], in0=xpre[:], scalar1=cw[:, 1, ch:ch + 1],
                         scalar2=cb[:, ch:ch + 1], op0=ALU.mult, op1=ALU.add)
                    for (t0, TT) in seqs:
                        e = t0 + TT
                        k.op("dve", "scalar_tensor_tensor", out=xc[:, mc, t0 + 1:e], in0=xpre[:, t0:e - 1],
                             scalar=cw[:, 0, ch:ch + 1], in1=xc[:, mc, t0 + 1:e], op0=ALU.mult, op1=ALU.add)
                        k.op("dve", "scalar_tensor_tensor", out=xc[:, mc, t0:e - 1], in0=xpre[:, t0 + 1:e],
                             scalar=cw[:, 2, ch:ch + 1], in1=xc[:, mc, t0:e - 1], op0=ALU.mult, op1=ALU.add)
                        k.op("dve", "scalar_tensor_tensor", out=xc[:, mc, t0:e - 2], in0=xpre[:, t0 + 2:e],
                             scalar=cw[:, 3, ch:ch + 1], in1=xc[:, mc, t0:e - 2], op0=ALU.mult, op1=ALU.add)
                    k.op("act", "activation", out=xcb[:, mc, :], in_=xc[:, mc, :], func=AF.Copy)
                for mc in range(4):
                    ch = gq * 4 + mc
                    bl, jc = mc // 2, mc % 2
                    for d in range(2):
                        for g_ in range(2):
                            dst = rt[d] if g_ == 0 else it[d]
                            for b in range(2):
                                blk = slice(b * 512, (b + 1) * 512)
                                pb = k.bank()
                                for kc in range(2):
                                    k.op("pe", "matmul", out=pb[:], lhsT=gw[:, bl, d * 2 + g_, kc, jc * 128:(jc + 1) * 128],
                                         rhs=xcb[:, bl * 2 + kc, blk], start=(kc == 0), stop=(kc == 1))
                                k.op("act", "activation", out=dst[:, blk], in_=pb[:], func=AF.Sigmoid,
                                     bias=gb_[:, d * 2 + g_, ch:ch + 1])
                    for d in range(2):
                        k.op("act", "activation", out=st_[d][:], in_=rt[d][:], func=AF.Exp, scale=c16[:, d, ch:ch + 1])
                        k.op("act", "activation", out=rt[d][:], in_=rt[d][:], func=AF.Exp, scale=c8[:, d, ch:ch + 1])
                    for d in range(2):
                        k.op("act", "activation", out=st_[d][:], in_=st_[d][:], func=AF.Sqrt, scale=-1.0, bias=1.0)
                    for d in range(2):
                        k.op("dve", "tensor_tensor", out=it[d][:], in0=it[d][:], in1=st_[d][:], op=ALU.mult)
                        k.op("dve", "tensor_tensor", out=it[d][:], in0=it[d][:], in1=xc[:, mc, :], op=ALU.mult)
                        for si, (t0, TT) in enumerate(seqs):
                            sl = slice(t0, t0 + TT)
                            init = h0[:, d, ch:ch + 1] if r == 0 else 0.0
                            if d == 0:
                                k.op("dve", "tensor_tensor_scan", out=hs[d][:, sl], data0=rt[d][:, sl], data1=it[d][:, sl],
                                     initial=init, op0=ALU.mult, op1=ALU.add)
                            else:
                                k.op("dve", "tensor_tensor_scan", out=hs[d][:, sl][:, ::-1], data0=rt[d][:, sl][:, ::-1],
                                     data1=it[d][:, sl][:, ::-1], initial=init, op0=ALU.mult, op1=ALU.add)
                            if r == 1:
                                idx = t0 + TT - 1 if d == 0 else t0
                                k.op("pool", "tensor_copy", out=hout[:, si, d, ch:ch + 1], in_=hs[d][:, idx:idx + 1])
                    k.op("dve", "tensor_tensor", out=hs[0][:], in0=hs[0][:], in1=hs[1][:], op=ALU.add)
                    k.op("dve", "tensor_tensor", out=yT[:, ch, :], in0=hs[0][:], in1=gbr[:, mc, :], op=ALU.mult)
            k.stage(load_g, comp_g)
            k.stage(load_x, comp_x)

        if r == 1:
            k.stage(None, lambda _: k.dma("sp", out=newh_o.rearrange("s d p c -> p s d c"), in_=hout[:]))

        def epi(pb, mc, b):
            blk = slice(b * 512, (b + 1) * 512)
            k.op("act", "activation", out=xT[:, mc, blk], in_=pb[:], func=AF.Copy)
            src_stat(mc, b, xT[:, mc, blk])
        linearT(lru_wout[0], D, D, yT, epi)
        return xT

    def mlstm(j, r, seqs):
        W = ml_win[j]
        vt = k.view(OFF_X, [128, 4, 8, 512], BF16)
        hn = k.view(OFF_X + 32 * KiB, [128, 4, 8, 512], BF16)
        yT = k.view(OFF_X, [128, KC, TOK], BF16)
        so = [OFF_S]

        def salloc(shape, dt):
            n = 1
            for s in shape[1:]:
                n *= s
            nb = n * _esz(dt)
            if nb >= 256:
                so[0] = (so[0] + 255) // 256 * 256
            v = k.view(so[0], shape, dt)
            so[0] += (nb + 31) // 32 * 32
            assert so[0] <= OFF_W, so[0]
            return v
        qT = salloc([128, 2, TOK], BF16)
        kT = salloc([128, 2, TOK], BF16)
        hsum = salloc([128, 8, 512], F32)
        Cst = [salloc([128, 2, 512], F32) for _ in range(2)]
        Cbf = salloc([128, 2, 512], BF16)
        Cbf_b = k.view(OFF_X + 62 * KiB, [128, 2, 512], BF16)
        Cbf2 = [Cbf, Cbf_b]
        nst = [salloc([128, 2], F32) for _ in range(2)]
        nbf = salloc([128, 2], BF16)
        nbf2 = [nbf, salloc([128, 2], BF16)]
        MB = salloc([128, TOK], F32)
        cols = salloc([128, 3, 2, 8, 4], F32)
        tn = salloc([128, 512], F32)
        Dm2 = [salloc([128, 128], F32) for _ in range(2)]
        PS2 = [salloc([128, 128], BF16) for _ in range(2)]
        kw2 = [salloc([128, 256], BF16) for _ in range(2)]
        small2 = [salloc([128, 16], F32) for _ in range(2)]
        m0bc = salloc([128, 8], F32)
        maskb = salloc([128, 2, 128], F32)
        ngc = salloc([128, 16], F32)
        ro = [OFF_X + 32 * KiB]

        def ralloc():
            v = k.view(ro[0], [4, TOK], F32)
            ro[0] += 4 * KiB
            assert ro[0] <= OFF_H
            return v
        G = [ralloc() for _ in range(4)]
        Fn = [ralloc() for _ in range(2)]
        Mr = [salloc([4, TOK], F32) for _ in range(2)]
        bg = salloc([4, 4], F32)
        m0t = salloc([4, 2], F32)
        mo = salloc([4, 4, 2], F32)
        sel4 = salloc([4, 4, 128], F32)
        id4 = salloc([4, 4], F32)
        wg = salloc([128, KC, 16], BF16)

        def s_init(_):
            k.dma("sp", out=bg[:], in_=ml_bg[j])
            k.dma("sp", out=sel4[:], in_=c_sel4.rearrange("h (a m) -> h a m", a=4))
            k.dma("sp", out=id4[:], in_=c_id4)
            k.dma("sp", out=maskb[:], in_=c_mask.rearrange("d s t -> s d t"))
            k.dma("sp", out=ngc[:], in_=ml_ng[j])
            k.dma("pool", out=wg[:], in_=W[:, 6144:6160].rearrange("(k p) m -> p k m", p=128))
            if r == 0:
                k.dma("sp", out=m0t[:], in_=stmT_d[j])
                for d in range(2):
                    for h in range(4):
                        k.dma("sp", out=m0bc[:, d * 4 + h: d * 4 + h + 1], in_=stm_d[j, d, h:h + 1].partition_broadcast(128))
            else:
                k.op("dve", "memset", ap=m0bc[:], constant=0.0)
            for cq in range(4):
                for b in range(2):
                    blk = slice(b * 512, (b + 1) * 512)
                    pb = k.bank()
                    for kc in range(KC):
                        k.op("pe", "matmul", out=pb[0:4, :], lhsT=wg[:, kc, cq * 4:(cq + 1) * 4], rhs=hinT[:, kc, blk],
                             start=(kc == 0), stop=(kc == KC - 1))
                    k.op("act", "activation", out=G[cq][:, blk], in_=pb[0:4, :], func=AF.Identity, bias=bg[:, cq:cq + 1])
            for d in range(2):
                li, lf = G[d * 2], G[d * 2 + 1]
                k.op("act", "activation", out=lf[:], in_=lf[:], func=AF.Exp, scale=-1.0)
                k.op("act", "activation", out=lf[:], in_=lf[:], func=AF.Ln, bias=1.0, scale=1.0)
                k.op("dve", "tensor_scalar", out=lf[:], in0=lf[:], scalar1=0.5, scalar2=None, op0=ALU.mult)
                for si, (t0, TT) in enumerate(seqs):
                    sl = slice(t0, t0 + TT)

                    def dv(ap):
                        return ap[:, sl] if d == 0 else ap[:, sl][:, ::-1]
                    k.op("dve", "tensor_tensor_scan", out=dv(Fn[d]), data0=dv(lf), data1=dv(lf), initial=0.0,
                         op0=ALU.add, op1=ALU.add)
                k.op("dve", "tensor_tensor", out=li[:], in0=li[:], in1=Fn[d][:], op=ALU.add)
                for si, (t0, TT) in enumerate(seqs):
                    sl = slice(t0, t0 + TT)

                    def dv(ap):
                        return ap[:, sl] if d == 0 else ap[:, sl][:, ::-1]
                    init = m0t[:, d:d + 1] if r == 0 else 0.0
                    k.op("dve", "tensor_tensor_scan", out=dv(Mr[d]), data0=dv(li), data1=dv(li), initial=init,
                         op0=ALU.max, op1=ALU.max)
                    if r == 1:
                        idx = t0 + TT - 1 if d == 0 else t0
                        k.op("dve", "tensor_tensor", out=mo[:, si, d:d + 1], in0=Mr[d][:, idx:idx + 1],
                             in1=Fn[d][:, idx:idx + 1], op=ALU.subtract)
                k.op("dve", "tensor_tensor", out=lf[:], in0=Fn[d][:], in1=Mr[d][:], op=ALU.subtract)
                k.op("act", "activation", out=lf[:], in_=lf[:], func=AF.Exp)
            pb = k.bank()
            for qi in range(3):
                for d in range(2):
                    src = (G[d * 2], Mr[d], G[d * 2 + 1])[qi]
                    for ti in range(8):
                        o = ((qi * 2 + d) * 8 + ti) * 4
                        k.op("pe", "matmul", out=pb[:, o:o + 4], lhsT=src[:, ti * 128:(ti + 1) * 128], rhs=id4[:],
                             start=True, stop=True)
            k.op("act", "activation", out=cols[:].rearrange("p a d t h -> p (a d t h)"), in_=pb[:, 0:192], func=AF.Copy)
            if r == 1:
                k.dma("sp", out=newm_o[j], in_=mo[:].rearrange("h s d -> h (s d)"))
        k.stage(None, s_init)
        if cfg.get('ml_stop', 99) <= 1:
            return outT_alt

        for h in range(4):
            def load_v(h=h):
                wb = k.wbuf([128, KC, 512])
                k.dma("pool", out=wb[:], in_=W[:, 2048 + h * 512: 2048 + (h + 1) * 512].rearrange("(k p) m -> p k m", p=128))
                return wb

            def comp_v(wb, h=h):
                for ti in range(8):
                    pb = k.bank()
                    for kc in range(KC):
                        k.op("pe", "matmul", out=pb[:], lhsT=hinT[:, kc, ti * 128:(ti + 1) * 128], rhs=wb[:, kc, :],
                             start=(kc == 0), stop=(kc == KC - 1))
                    if ti % 2 == 0:
                        k.op("act", "activation", out=vt[:, h, ti, :], in_=pb[:], func=AF.Copy)
                    else:
                        k.op("dve", "tensor_copy", out=vt[:, h, ti, :], in_=pb[:])
            k.stage(load_v, comp_v)

        if cfg.get('ml_stop', 99) <= 2:
            return outT_alt
        mb_off = [kk for kk, vv in k.views.items() if vv is MB][0][0]
        cst_off = [kk for kk, vv in k.views.items() if vv is Cst[0]][0][0]
        RB = [k.banks[5], k.banks[6], k.banks[7]]
        rot = [0]
        nheads = cfg.get('ml_heads', 4)

        def o_tile(wb, h, ti, loc, n):
            sg = k.view(loc + 2 * KiB, [128, 512], F32)
            yb = k.view(loc + (ti % 2) * KiB, [128, 512], BF16)
            pb = k.bank()
            for kc in range(KC):
                k.op("pe", "matmul", out=pb[:], lhsT=hinT[:, kc, ti * 128:(ti + 1) * 128], rhs=wb[:, kc, :],
                     start=(kc == 0), stop=(kc == KC - 1))
            k.op("act", "activation", out=sg[:], in_=pb[:], func=AF.Sigmoid)
            k.op("dve", "tensor_tensor", out=yb[:], in0=sg[:], in1=hn[:, h, ti, :], op=ALU.mult)
            pbt = k.bank_bf()
            for c in range(4):
                k.op("pe", "transpose", out=pbt[:, c * 128:(c + 1) * 128], in_=yb[:, c * 128:(c + 1) * 128], identity=ident[:])
            src = pbt[:, 0:512].rearrange("p (c t) -> p c t", c=4)
            dst = yT[:, h * 4:(h + 1) * 4, ti * 128:(ti + 1) * 128]
            if n % 2 == 0:
                k.op("act", "activation", out=dst, in_=src, func=AF.Copy)
            else:
                k.op("dve", "tensor_copy", out=dst, in_=src)

        def recur(h, wb_o):
            steps = []
            for d in range(2):
                for si, (t0, TT) in enumerate(seqs):
                    nt = TT // 128
                    tl0 = t0 // 128
                    order = list(range(nt)) if d == 0 else list(range(nt - 1, -1, -1))
                    for oi, tl in enumerate(order):
                        steps.append(dict(d=d, si=si, ti=tl0 + tl, first=(oi == 0), last=(oi == nt - 1),
                                          newd=(si == 0 and oi == 0)))
            nsteps = len(steps)
            cur = {}

            def pre(g):
                s_ = steps[g]
                d, ti = s_['d'], s_['ti']
                par = g % 2
                sm = small2[par]
                if s_['newd']:
                    for b in range(2):
                        blk = slice(b * 512, (b + 1) * 512)
                        pb = k.bank()
                        k.op("pe", "matmul", out=pb[:], lhsT=sel4[:, h, :], rhs=Mr[d][:, blk], start=True, stop=True)
                        k.op("act", "activation", out=MB[:, blk], in_=pb[:], func=AF.Copy)
                tsl = slice(ti * 128, (ti + 1) * 128)
                have_state = (r == 0) or (not s_['first'])
                need_update = (r == 1) or (not s_['last'])
                acol = cols[:, 0, d, ti, h:h + 1]
                mcol = cols[:, 1, d, ti, h:h + 1]
                if s_['first']:
                    mprev = m0bc[:, d * 4 + h: d * 4 + h + 1]
                else:
                    pi = (ti * 128 - 1) if d == 0 else ((ti + 1) * 128)
                    mprev = MB[:, pi:pi + 1]
                ei = ((ti + 1) * 128 - 1) if d == 0 else (ti * 128)
                mend = MB[:, ei:ei + 1]
                pss = k.bank()
                for c in range(2):
                    k.op("pe", "matmul", out=pss[:, 0:128], lhsT=kT[:, c, tsl], rhs=qT[:, c, tsl], start=(c == 0), stop=(c == 1))
                Dm, PS, kw = Dm2[par], PS2[par], kw2[par]
                k.op("dve", "scalar_tensor_tensor", out=Dm[:], in0=MB[:, tsl], scalar=acol, in1=maskb[:, d, :],
                     op0=ALU.subtract, op1=ALU.max)
                k.op("act", "activation", out=Dm[:], in_=Dm[:], func=AF.Exp, scale=-1.0)
                k.op("dve", "tensor_tensor", out=PS[:], in0=Dm[:], in1=pss[:, 0:128], op=ALU.mult)
                psn = RB[par]
                psd = RB[2][:, par * 4: par * 4 + 2]
                k.op("pe", "matmul", out=psn[:], lhsT=PS[:], rhs=vt[:, h, ti, :], start=True, stop=True)
                k.op("pe", "matmul", out=psd[:, 0:1], lhsT=PS[:], rhs=ones1[:, 0:1], start=True, stop=True)
                if have_state:
                    k.op("act", "activation", out=sm[:, 0:1], in_=mcol, func=AF.Exp, scale=-1.0, bias=mprev)
                if need_update:
                    k.op("act", "activation", out=sm[:, 8:9], in_=mend, func=AF.Exp, scale=-1.0, bias=acol)
                    k.op("act", "activation", out=sm[:, 9:10], in_=mend, func=AF.Exp, scale=-1.0, bias=mprev)
                    pbt = k.bank_bf()
                    for c in range(2):
                        k.op("pe", "transpose", out=pbt[:, c * 128:(c + 1) * 128], in_=kT[:, c, tsl], identity=ident[:])
                    k.op("act", "activation", out=kw[:], in_=pbt[:, 0:256], func=AF.Identity, scale=sm[:, 8:9])

            def post(g):
                s_ = steps[g]
                d, ti, si = s_['d'], s_['ti'], s_['si']
                par = g % 2
                sm = small2[par]
                kw = kw2[par]
                tsl = slice(ti * 128, (ti + 1) * 128)
                first, last = s_['first'], s_['last']
                have_state = (r == 0) or (not first)
                need_update = (r == 1) or (not last)
                emcol = cols[:, 2, d, ti, h:h + 1]
                psn = RB[par]
                psd = RB[2][:, par * 4: par * 4 + 2]
                if first:
                    cur['C'] = Cst[rot[0] % 2]
                    cur['n'] = nst[rot[0] % 2]
                    rot[0] += 1
                    cur.setdefault('v', 0)
                    C, nn = cur['C'], cur['n']
                    if r == 0:
                        k.dma("sp", out=C[:], in_=stC_d[j, d, h].rearrange("(c p) v -> p c v", p=128))
                        k.dma("sp", out=nn[:], in_=stn_d[j, d, h])
                        k.op("act", "activation", out=Cbf2[cur['v']][:], in_=C[:], func=AF.Copy)
                        k.op("act", "activation", out=nbf2[cur['v']][:], in_=nn[:], func=AF.Copy)
                    else:
                        k.op("dve", "memset", ap=C[:], constant=0.0)
                        k.op("dve", "memset", ap=nn[:], constant=0.0)
                C, nn = cur['C'], cur['n']
                v = cur['v']
                if need_update:
                    psc = [k.bank(), k.bank()]
                    for c in range(2):
                        k.op("pe", "matmul", out=psc[c][:], lhsT=kw[:, c * 128:(c + 1) * 128], rhs=vt[:, h, ti, :],
                             start=True, stop=True)
                    psdn = RB[2][:, 8:10]
                    for c in range(2):
                        k.op("pe", "matmul", out=psdn[:, c:c + 1], lhsT=kw[:, c * 128:(c + 1) * 128], rhs=ones1[:, 0:1],
                             start=True, stop=True)
                if have_state:
                    psi = k.bank()
                    for c in range(2):
                        k.op("pe", "matmul", out=psi[:], lhsT=qT[:, c, tsl], rhs=Cbf2[v][:, c, :], start=(c == 0), stop=(c == 1))
                    for c in range(2):
                        k.op("pe", "matmul", out=psd[:, 1:2], lhsT=qT[:, c, tsl], rhs=nbf2[v][:, c:c + 1], start=(c == 0), stop=(c == 1))
                if need_update:
                    for c in range(2):
                        k.op("dve", "scalar_tensor_tensor", out=C[:, c, :], in0=C[:, c, :], scalar=sm[:, 9:10],
                             in1=psc[c][:], op0=ALU.mult, op1=ALU.add)
                    k.op("dve", "scalar_tensor_tensor", out=nn[:], in0=nn[:], scalar=sm[:, 9:10],
                         in1=psdn[:, 0:2], op0=ALU.mult, op1=ALU.add)
                    if not last:
                        k.op("act", "activation", out=Cbf2[1 - v][:], in_=C[:], func=AF.Copy)
                        k.op("act", "activation", out=nbf2[1 - v][:], in_=nn[:], func=AF.Copy)
                        cur['v'] = 1 - v
                if have_state:
                    k.op("act", "activation", out=sm[:, 1:3], in_=psd[:, 0:2], func=AF.Copy)
                    k.op("dve", "scalar_tensor_tensor", out=sm[:, 3:4], in0=sm[:, 2:3], scalar=sm[:, 0:1],
                         in1=sm[:, 1:2], op0=ALU.mult, op1=ALU.add)
                else:
                    k.op("act", "activation", out=sm[:, 3:4], in_=psd[:, 0:1], func=AF.Copy)
                k.op("dve", "scalar_tensor_tensor", out=sm[:, 4:5], in0=sm[:, 3:4], scalar=-1.0, in1=sm[:, 3:4], op0=ALU.mult, op1=ALU.max)
                k.op("dve", "tensor_tensor", out=sm[:, 4:5], in0=sm[:, 4:5], in1=emcol, op=ALU.max)
                k.op("dve", "reciprocal", out=sm[:, 5:6], in_=sm[:, 4:5])
                if have_state:
                    k.op("dve", "tensor_tensor", out=sm[:, 6:7], in0=sm[:, 5:6], in1=sm[:, 0:1], op=ALU.mult)
                    k.op("act", "activation", out=tn[:], in_=psn[:], func=AF.Identity, scale=sm[:, 5:6])
                    if d == 0:
                        k.op("dve", "scalar_tensor_tensor", out=hsum[:, ti, :], in0=psi[:], scalar=sm[:, 6:7],
                             in1=tn[:], op0=ALU.mult, op1=ALU.add)
                    else:
                        k.op("dve", "scalar_tensor_tensor", out=tn[:], in0=psi[:], scalar=sm[:, 6:7],
                             in1=tn[:], op0=ALU.mult, op1=ALU.add)
                        k.op("dve", "tensor_tensor", out=hsum[:, ti, :], in0=hsum[:, ti, :], in1=tn[:], op=ALU.add)
                else:
                    if d == 0:
                        k.op("act", "activation", out=hsum[:, ti, :], in_=psn[:], func=AF.Identity, scale=sm[:, 5:6])
                    else:
                        k.op("dve", "scalar_tensor_tensor", out=hsum[:, ti, :], in0=psn[:], scalar=sm[:, 5:6],
                             in1=hsum[:, ti, :], op0=ALU.mult, op1=ALU.add)
                if last and r == 1:
                    k.dma("sp", out=newC_o[si, j, d, h].rearrange("(c p) v -> p c v", p=128), in_=C[:])
                    k.dma("sp", out=newn_o[si, j, d, h], in_=nn[:])

            k.nbanks = 3
            OB = k.banks[4]
            OT = k.as_bf(k.banks[3])
            loc = OFF_X + 56 * KiB
            sg = k.view(loc + 2 * KiB, [128, 512], F32)

            def o_stage(g):
                if wb_o is None:
                    return
                hh = h - 1
                if g >= 3 and (g - 3) % 2 == 0 and (g - 3) // 2 < 8:
                    n = (g - 3) // 2
                    k.op("act", "activation", out=yT[:, hh * 4:(hh + 1) * 4, n * 128:(n + 1) * 128],
                         in_=OT[:, 0:512].rearrange("p (c t) -> p c t", c=4), func=AF.Copy)
                if g >= 2 and (g - 2) % 2 == 0 and (g - 2) // 2 < 8:
                    n = (g - 2) // 2
                    yb = k.view(loc + (n % 2) * KiB, [128, 512], BF16)
                    for c in range(4):
                        k.op("pe", "transpose", out=OT[:, c * 128:(c + 1) * 128], in_=yb[:, c * 128:(c + 1) * 128], identity=ident[:])
                if g >= 1 and (g - 1) % 2 == 0 and (g - 1) // 2 < 8:
                    n = (g - 1) // 2
                    yb = k.view(loc + (n % 2) * KiB, [128, 512], BF16)
                    k.op("act", "activation", out=sg[:], in_=OB[:], func=AF.Exp, scale=-1.0)
                    k.op("act", "activation", out=sg[:], in_=sg[:], func=AF.Ln, bias=1.0, scale=1.0)
                    k.op("act", "activation", out=sg[:], in_=sg[:], func=AF.Exp, scale=-1.0)
                    k.op("pool", "tensor_tensor", out=yb[:], in0=sg[:], in1=hn[:, hh, n, :], op=ALU.mult)
                if g % 2 == 0 and g // 2 < 8:
                    n = g // 2
                    for kc in range(KC):
                        k.op("pe", "matmul", out=OB[:], lhsT=hinT[:, kc, n * 128:(n + 1) * 128], rhs=wb_o[:, kc, :],
                             start=(kc == 0), stop=(kc == KC - 1))

            pre(0)
            for g in range(nsteps):
                if g + 1 < nsteps:
                    pre(g + 1)
                post(g)
                o_stage(g)
            for g in range(nsteps, nsteps + 4):
                o_stage(g)
            k.nbanks = 8
            if h < 3:
                ngb = k.view(OFF_X + 60 * KiB, [128, 512], F32)
            else:
                ngb = k.view(cst_off, [128, 512], F32)
            k.dma("sp", out=ngb[:], in_=ml_ngrow[j, h * 512:(h + 1) * 512].partition_broadcast(128))
            sm = small2[0]
            for ti in range(8):
                k.op("act", "activation", out=tn[:], in_=hsum[:, ti, :], func=AF.Square, accum_out=sm[:, 10:11])
                k.op("act", "activation", out=sm[:, 11:12], in_=sm[:, 10:11], func=AF.Ln, scale=1.0 / 512.0, bias=EPS)
                k.op("act", "activation", out=sm[:, 12:13], in_=sm[:, 11:12], func=AF.Exp, scale=-0.5)
                k.op("dve", "scalar_tensor_tensor", out=hn[:, h, ti, :], in0=hsum[:, ti, :], scalar=sm[:, 12:13],
                     in1=ngb[:], op0=ALU.mult, op1=ALU.mult)

        def load_o_fn(h):
            def load_o():
                wb = k.wbuf([128, KC, 512])
                k.dma("pool", out=wb[:], in_=W[:, 4096 + h * 512: 4096 + (h + 1) * 512].rearrange("(k p) m -> p k m", p=128))
                return wb
            return load_o

        for h in range(nheads):
            def load_qk(h=h):
                wb = k.wbuf([128, KC, 512])
                k.dma("pool", out=wb[:, :, 0:256], in_=W[:, h * 256:(h + 1) * 256].rearrange("(k p) m -> p k m", p=128))
                k.dma("pool", out=wb[:, :, 256:512], in_=W[:, 1024 + h * 256: 1024 + (h + 1) * 256].rearrange("(k p) m -> p k m", p=128))
                return wb

            def comp_qk(wb, h=h):
                for mc in range(4):
                    for b in range(2):
                        blk = slice(b * 512, (b + 1) * 512)
                        pb = k.bank()
                        for kc in range(KC):
                            k.op("pe", "matmul", out=pb[:], lhsT=wb[:, kc, mc * 128:(mc + 1) * 128], rhs=hinT[:, kc, blk],
                                 start=(kc == 0), stop=(kc == KC - 1))
                        if mc < 2:
                            k.op("act", "activation", out=qT[:, mc, blk], in_=pb[:], func=AF.Copy)
                        else:
                            k.op("act", "activation", out=kT[:, mc - 2, blk], in_=pb[:], func=AF.Identity, scale=1.0 / 16.0)
            k.stage(load_qk, comp_qk)
            if h == 0:
                k.stage(None, lambda _, h=h: recur(h, None))
            else:
                k.stage(load_o_fn(h - 1), lambda wb, h=h: recur(h, wb))

        def comp_o_last(wb):
            k.nbanks = 8
            for ti in range(8):
                o_tile(wb, nheads - 1, ti, mb_off, ti)
        k.stage(load_o_fn(nheads - 1), comp_o_last)

        def epi(pb, mc, b):
            blk = slice(b * 512, (b + 1) * 512)
            k.op("act", "activation", out=outT_alt[:, mc, blk], in_=pb[:], func=AF.Copy)
            src_stat(mc, b, outT_alt[:, mc, blk])
        linearT(ml_wout[j], D, D, yT, epi)
        return outT_alt

    for r in passes:
        seqs = [(0, 1024)] if r == 0 else [(i * 256, 256) for i in range(4)]
        k.stage(None, lambda _, r=r: k.dma("sp", out=xT[:], in_=xT_in[r].rearrange("(c p) t -> p c t", p=128)))
        for l in range(nlayers):
            def pre1(_, l=l, r=r):
                derive(l, r)
                prenorm(l, r, 0, have_stats=(l > 0))
                spill()
            k.stage(None, pre1)
            kind, j = l % 3, l // 3
            if cfg.get('skip_mixer'):
                k.stage(None, lambda _: None)
                src = xT
            elif kind == 0:
                src = mlstm(j, r, seqs)
            elif kind == 1:
                src = fnet(r, seqs)
            else:
                src = rglru(r, seqs)
            last_sub = stop_after_mixer and l == nlayers - 1
            k.stage(None, lambda _, src=src, f=(not last_sub): postnorm(src, 1, fuse_next=f, have_src_stats=not cfg.get('skip_mixer')))
            if last_sub:
                break

            def pre2(_, l=l, r=r):
                prenorm(l, r, 1, have_stats=True)
                spill()
            k.stage(None, pre2)
            ex = None
            if r == passes[0] and l + 1 < nlayers and not cfg.get('skip_mod'):
                ex = mod_stages(l + 1)
            ffn(l, ex)
            k.stage(None, lambda _, f=(l + 1 < nlayers): postnorm(xT, 3, fuse_next=f, have_src_stats=True))
        k.stage(None, lambda _, r=r: k.dma("sp", out=yT_out[r].rearrange("(c p) t -> p c t", p=128), in_=xT[:]))
    k.run(depth=2)
    k.finish()
    return k


def _fm(v):
    s = v.shape
    return np.ascontiguousarray(v.reshape(s[:-1] + (s[-1] // 128, 128)).swapaxes(-1, -2))


def _consts():
    c = {}
    c["c_ident"] = np.eye(128, dtype=np.float32)
    i = np.arange(256)
    ang = 2.0 * np.pi * ((i[:, None] * i[None, :]) % 256) / 256.0
    c["c_cs256"] = np.concatenate([np.cos(ang), np.sin(ang)], axis=1).astype(np.float32)
    for T in (1024, 256):
        t = np.arange(T)
        ang = 2.0 * np.pi * ((t[:, None] * t[None, :]) % T) / float(T)
        c[f"c_dft{T}"] = np.concatenate([np.cos(ang), -np.sin(ang)], axis=1).astype(np.float32)
    s = np.arange(128)
    m0 = np.where(s[:, None] <= s[None, :], 0.0, 30000.0)
    m1 = np.where(s[:, None] >= s[None, :], 0.0, 30000.0)
    c["c_mask"] = np.stack([m0, m1]).astype(np.float32)
    sel = np.zeros((4, 4, 128), np.float32)
    for h in range(4):
        sel[h, h, :] = 1.0
    c["c_sel4"] = sel.reshape(4, 512)
    c["c_id4"] = np.eye(4, dtype=np.float32)
    return c


_CACHE = {}


def _program(cfg=None):
    kx = repr(sorted((cfg or {}).items()))
    if kx not in _CACHE:
        _CACHE[kx] = build_program(cfg)
    return _CACHE[kx]


def make_in_maps(inp, ncores=8, cfg=None):
    f = lambda a: np.ascontiguousarray(np.asarray(a, dtype=np.float32))
    shared = {
        "mod_w": f(inp["mod_w"]),
        "mod_b_fm": f(_fm(f(inp["mod_b"])).transpose(1, 0, 2).reshape(128, 4 * 96)),
        "norm_g_fm": f(_fm(f(inp["norm_g"])).transpose(2, 0, 1, 3).reshape(128, 4 * 4 * 16)),
        "ffn_w_up": f(inp["ffn_w_up"]),
        "ffn_w_down": f(inp["ffn_w_down"]),
        "mlstm_w_in": f(inp["mlstm_w_in"]),
        "mlstm_bg": f(f(inp["mlstm_b_gate"]).reshape(2, 4, 4).transpose(0, 2, 1)),
        "mlstm_ng_fm": f(_fm(f(inp["mlstm_norm_g"]))),
        "mlstm_ng_row": f(inp["mlstm_norm_g"]),
        "mlstm_w_out": f(inp["mlstm_w_out"]),
        "fnet_w_out": f(inp["fnet_w_out"]),
        "fnet_b_fm": f(_fm(f(inp["fnet_b_out"])[0])),
        "lru_w_in": f(inp["lru_w_in"]),
        "lru_cw_fm": f(_fm(f(inp["lru_conv_w"])[0]).transpose(1, 0, 2).reshape(128, 64)),
        "lru_cb_fm": f(_fm(f(inp["lru_conv_b"])[0])),
        "lru_gate_w": f(inp["lru_gate_w"]),
        "lru_gb_fm": f(_fm(f(inp["lru_gate_b"])[0].reshape(4, D)).transpose(1, 0, 2).reshape(128, 64)),
        "lru_lam_fm": f(_fm(f(inp["lru_lambda"])[0]).transpose(1, 0, 2).reshape(128, 32)),
        "lru_w_out": f(inp["lru_w_out"]),
    }
    shared.update(_consts())
    xs, xp = f(inp["x_sample"]), f(inp["x_prompt"])
    cc, cctx = f(inp["c"]), f(inp["c_ctx"])
    sC, sn, sm, sh = f(inp["state_mlstm_C"]), f(inp["state_mlstm_n"]), f(inp["state_mlstm_m"]), f(inp["state_lru_h"])
    maps = []
    for i in range(ncores):
        m = dict(shared)
        m["xT_a"] = f(xs[i].T)
        m["xT_b"] = f(xp[4 * i:4 * i + 4].reshape(TOK, D).T)
        m["cond"] = f(np.stack([_fm(cc[i]), _fm(cctx)]))
        m["stC"] = f(sC[i])
        m["stn"] = f(sn[i].reshape(2, 2, 4, 2, 128).swapaxes(-1, -2))
        m["stm"] = f(sm[i])
        m["stmT"] = f(sm[i].transpose(0, 2, 1))
        m["sth"] = f(_fm(sh[i, 0]))
        if cfg and cfg.get('slim'):
            for kx, shp in _big_shapes(cfg).items():
                m[kx] = _slice_to(m[kx], shp)
        maps.append(m)
    return maps


def gather(results, ncores=8):
    y_p = np.zeros((32, 256, D), np.float32)
    y_s = np.zeros((8, 1024, D), np.float32)
    nC = np.zeros((32, 2, 2, 4, 256, 512), np.float32)
    nn = np.zeros((32, 2, 2, 4, 256), np.float32)
    nm = np.zeros((32, 2, 2, 4), np.float32)
    nh = np.zeros((32, 1, 2, D), np.float32)
    for i in range(ncores):
        r = results[i]
        y_s[i] = r["yT_a"].T
        y_p[4 * i:4 * i + 4] = r["yT_b"].T.reshape(4, 256, D)
        nC[4 * i:4 * i + 4] = r["newC"]
        nn[4 * i:4 * i + 4] = r["newn"].swapaxes(-1, -2).reshape(4, 2, 2, 4, 256)
        nm[4 * i:4 * i + 4] = r["newm"].reshape(2, 4, 4, 2).transpose(2, 0, 3, 1)
        nh[4 * i:4 * i + 4, 0] = r["newh"].swapaxes(-1, -2).reshape(4, 2, D)
    return (y_p, y_s, nC, nn, nm, nh)


def kernel(**inputs):
    nc = _program()
    maps = make_in_maps(inputs)
    res = run_bass_kernel_spmd(nc, maps, core_ids=list(range(8)))
    return gather(res.results)
```
